# Optimizing a Trainium2 kernel written in Bass

```python
import math
import jax, jax.numpy as jnp
from jax import lax
import numpy as np

D_MODEL = 1024
BATCH = 32
SEQ = 2048
DEPTH = 1

MOBA_HEADS = 8
MOBA_HEAD_DIM = 64
MOBA_WIDTH = MOBA_HEADS * MOBA_HEAD_DIM
MOBA_ROT_DIM = MOBA_HEAD_DIM // 4
MOBA_BLOCK = 256
MOBA_TOPK = 3
MOBA_Q_CHUNK = 128
MLA_HEADS = 8
MLA_NOPE = 64
MLA_ROPE = 32
MLA_V = 64
MLA_Q_LORA = 384
MLA_KV_LORA = 256
MLA_WIDTH = MLA_HEADS * MLA_V
MLA_Q_BLOCK = 128
ROPE_THETA = 500000.0
D_FF = 4 * D_MODEL
EPS = 1e-6
IN_WIDTH = 3 * MOBA_WIDTH + MLA_Q_LORA + MLA_KV_LORA + MLA_ROPE + 2 * D_MODEL

kernel_name = "hybrid_moba_mla_gated_block"


def _rms_norm(x, g):
    xf = x.astype(jnp.float32)
    y = xf * lax.rsqrt(jnp.mean(xf * xf, axis=-1, keepdims=True) + EPS)
    return (y * g.astype(jnp.float32)).astype(x.dtype)


def _split_cols(t, widths):
    out, start = [], 0
    for w in widths:
        out.append(t[..., start:start + w])
        start += w
    return out


def _rope(x, positions):
    r = x.shape[-1]
    half = r // 2
    inv_freq = ROPE_THETA ** (-jnp.arange(half, dtype=jnp.float32) * (2.0 / r))
    ang = positions.astype(jnp.float32)[..., None] * inv_freq
    cos = jnp.cos(ang)[:, :, None, :]
    sin = jnp.sin(ang)[:, :, None, :]
    xf = x.astype(jnp.float32)
    x1, x2 = xf[..., :half], xf[..., half:]
    return jnp.concatenate([x1 * cos - x2 * sin, x2 * cos + x1 * sin], axis=-1).astype(x.dtype)


def _partial_rope(x, positions):
    return jnp.concatenate([_rope(x[..., :MOBA_ROT_DIM], positions), x[..., MOBA_ROT_DIM:]], axis=-1)


def _moba_attention(q, k, v):
    B, S, H, Dh = q.shape
    nb = -(-S // MOBA_BLOCK)
    pad = nb * MOBA_BLOCK - S
    topk = min(MOBA_TOPK, nb)
    n_chunks = S // MOBA_Q_CHUNK
    scale = Dh ** -0.5
    qt = jnp.transpose(q, (0, 2, 1, 3))
    kp = jnp.pad(jnp.transpose(k, (0, 2, 1, 3)), ((0, 0), (0, 0), (0, pad), (0, 0)))
    vp = jnp.pad(jnp.transpose(v, (0, 2, 1, 3)), ((0, 0), (0, 0), (0, pad), (0, 0)))
    kb = kp.reshape(B, H, nb, MOBA_BLOCK, Dh)
    vb = vp.reshape(B, H, nb, MOBA_BLOCK, Dh)
    kmean = jnp.mean(kb.astype(jnp.float32), axis=3)
    head_idx = jnp.arange(H)[:, None, None]

    def per_batch(args):
        qb, kbb, vbb, kmb = args

        def per_chunk(c):
            q0 = c * MOBA_Q_CHUNK
            qc = lax.dynamic_slice_in_dim(qb, q0, MOBA_Q_CHUNK, axis=1)
            own = q0 // MOBA_BLOCK
            gate = jnp.einsum('hqd,hnd->hqn', qc.astype(jnp.float32), kmb)
            gate = jnp.where(jnp.arange(nb)[None, None, :] < own, gate, -jnp.inf)
            _, idx = lax.top_k(gate, topk)
            valid = jnp.arange(topk) < own
            kg = kbb[head_idx, idx]
            vg = vbb[head_idx, idx]
            s_past = jnp.einsum('hqd,hqnkd->hqnk', qc, kg).astype(jnp.float32) * scale
            s_past = jnp.where(valid[None, None, :, None], s_past, -jnp.inf)
            s_past = s_past.reshape(H, MOBA_Q_CHUNK, topk * MOBA_BLOCK)
            k_own = lax.dynamic_index_in_dim(kbb, own, axis=1, keepdims=False)
            v_own = lax.dynamic_index_in_dim(vbb, own, axis=1, keepdims=False)
            s_own = jnp.einsum('hqd,hkd->hqk', qc, k_own).astype(jnp.float32) * scale
            qpos = q0 + jnp.arange(MOBA_Q_CHUNK)
            kpos = own * MOBA_BLOCK + jnp.arange(MOBA_BLOCK)
            s_own = jnp.where(kpos[None, None, :] <= qpos[None, :, None], s_own, -jnp.inf)
            p = jax.nn.softmax(jnp.concatenate([s_past, s_own], axis=-1), axis=-1).astype(v.dtype)
            p_past = p[..., :topk * MOBA_BLOCK].reshape(H, MOBA_Q_CHUNK, topk, MOBA_BLOCK)
            p_own = p[..., topk * MOBA_BLOCK:]
            return (jnp.einsum('hqnk,hqnkd->hqd', p_past, vg)
                    + jnp.einsum('hqk,hkd->hqd', p_own, v_own))

        outs = lax.map(per_chunk, jnp.arange(n_chunks))
        return jnp.transpose(outs, (1, 0, 2, 3)).reshape(H, S, Dh)

    o = lax.map(per_batch, (qt, kb, vb, kmean))
    return jnp.transpose(o, (0, 2, 1, 3))


def _mla_attention(q_nope, q_rope, k_nope, k_rope, v):
    S = q_nope.shape[1]
    scale = (MLA_NOPE + MLA_ROPE) ** -0.5
    outs = []
    for c in range(S // MLA_Q_BLOCK):
        q0, q1 = c * MLA_Q_BLOCK, (c + 1) * MLA_Q_BLOCK
        s = (jnp.einsum('bqhd,bkhd->bhqk', q_nope[:, q0:q1], k_nope[:, :q1])
             + jnp.einsum('bqhr,bkr->bhqk', q_rope[:, q0:q1], k_rope[:, :q1]))
        s = s.astype(jnp.float32) * scale
        mask = jnp.arange(q1)[None, :] <= (q0 + jnp.arange(MLA_Q_BLOCK))[:, None]
        s = jnp.where(mask[None, None], s, -jnp.inf)
        p = jax.nn.softmax(s, axis=-1).astype(v.dtype)
        outs.append(jnp.einsum('bhqk,bkhd->bqhd', p, v[:, :q1]))
    return jnp.concatenate(outs, axis=1)


def setup_inputs(seed: int = 0) -> dict:
    key = jax.random.key(seed)
    ks = jax.random.split(key, 20)
    f32 = jnp.float32

    def w(k, shape, fan_in):
        return jax.random.normal(k, shape, f32) * (fan_in ** -0.5)

    def gain(k, n):
        return 1.0 + 0.1 * jax.random.normal(k, (DEPTH, n), f32)

    x = jax.random.normal(ks[0], (BATCH, SEQ, D_MODEL), f32)
    offsets = jax.random.randint(ks[1], (BATCH, 1), 0, 4096)
    positions = (offsets + jnp.arange(SEQ)[None, :]).astype(jnp.int32)
    return {
        "x": x,
        "positions": positions,
        "g_pre_mix": gain(ks[2], D_MODEL),
        "w_in": w(ks[3], (DEPTH, D_MODEL, IN_WIDTH), D_MODEL),
        "b_gate": 0.01 * jax.random.normal(ks[4], (DEPTH, 2 * D_MODEL), f32),
        "g_q_norm": gain(ks[5], MLA_Q_LORA),
        "w_uq": w(ks[6], (DEPTH, MLA_Q_LORA, MLA_HEADS * (MLA_NOPE + MLA_ROPE)), MLA_Q_LORA),
        "g_kv_norm": gain(ks[7], MLA_KV_LORA),
        "w_ukv": w(ks[8], (DEPTH, MLA_KV_LORA, MLA_HEADS * (MLA_NOPE + MLA_V)), MLA_KV_LORA),
        "w_branch_a": w(ks[9], (DEPTH, MOBA_WIDTH, D_MODEL), MOBA_WIDTH),
        "w_branch_b": w(ks[10], (DEPTH, MLA_WIDTH, D_MODEL), MLA_WIDTH),
        "w_out": w(ks[11], (DEPTH, D_MODEL, D_MODEL), D_MODEL),
        "g_post_mix": gain(ks[12], D_MODEL),
        "g_pre_mlp": gain(ks[13], D_MODEL),
        "w_up": w(ks[14], (DEPTH, D_MODEL, D_FF), D_MODEL),
        "w_down": w(ks[15], (DEPTH, D_FF, D_MODEL), D_FF),
        "g_post_mlp": gain(ks[16], D_MODEL),
    }


def reference(x, positions, g_pre_mix, w_in, b_gate, g_q_norm, w_uq, g_kv_norm, w_ukv,
              w_branch_a, w_branch_b, w_out, g_post_mix, g_pre_mlp, w_up, w_down, g_post_mlp):
    B, S, _ = x.shape
    for l in range(DEPTH):
        h = _rms_norm(x, g_pre_mix[l])
        proj = h @ w_in[l]
        qa, ka, va, c_q, c_kv, k_r, gate_logits = _split_cols(
            proj, [MOBA_WIDTH, MOBA_WIDTH, MOBA_WIDTH, MLA_Q_LORA, MLA_KV_LORA, MLA_ROPE, 2 * D_MODEL])
        qa = _partial_rope(qa.reshape(B, S, MOBA_HEADS, MOBA_HEAD_DIM), positions)
        ka = _partial_rope(ka.reshape(B, S, MOBA_HEADS, MOBA_HEAD_DIM), positions)
        va = va.reshape(B, S, MOBA_HEADS, MOBA_HEAD_DIM)
        o_a = _moba_attention(qa, ka, va).reshape(B, S, MOBA_WIDTH) @ w_branch_a[l]
        q = (_rms_norm(c_q, g_q_norm[l]) @ w_uq[l]).reshape(B, S, MLA_HEADS, MLA_NOPE + MLA_ROPE)
        q_nope = q[..., :MLA_NOPE]
        q_rope = _rope(q[..., MLA_NOPE:], positions)
        kv = (_rms_norm(c_kv, g_kv_norm[l]) @ w_ukv[l]).reshape(B, S, MLA_HEADS, MLA_NOPE + MLA_V)
        k_nope, v_b = kv[..., :MLA_NOPE], kv[..., MLA_NOPE:]
        k_rope = _rope(k_r[:, :, None, :], positions)[:, :, 0, :]
        o_b = _mla_attention(q_nope, q_rope, k_nope, k_rope, v_b).reshape(B, S, MLA_WIDTH) @ w_branch_b[l]
        gates = jax.nn.sigmoid((gate_logits + b_gate[l]).astype(jnp.float32)).astype(x.dtype)
        merged = gates[..., :D_MODEL] * o_a + gates[..., D_MODEL:] * o_b
        x = x + _rms_norm(merged @ w_out[l], g_post_mix[l])
        h2 = _rms_norm(x, g_pre_mlp[l])
        m = jnp.square(jax.nn.relu(h2 @ w_up[l])) @ w_down[l]
        x = x + _rms_norm(m, g_post_mlp[l])
    return x
```

```python
import math
from contextlib import ExitStack
from functools import partial

import numpy as np
import concourse.bass as bass
import concourse.mybir as mybir
from concourse.bass_utils import run_bass_kernel_spmd

F32 = mybir.dt.float32
BF16 = mybir.dt.bfloat16
I32 = mybir.dt.int32
ALU = mybir.AluOpType
AF = mybir.ActivationFunctionType
AX = mybir.AxisListType

NCORES = 8
NSEQ = 4
S = 2048
D = 1024
EPS = 1e-6
NEG = -30000.0
THETA = 500000.0


class _Stop(Exception):
    pass


_STOP = [None, None, None]


def _ck(name):
    if _STOP[0] == name:
        sc = _STOP[1]
        sc.barrier()
        build_nc.stats = sc.emit()
        raise _Stop()


class Buf:
    __slots__ = ("name", "last_w", "readers")

    def __init__(self, name):
        self.name = name
        self.last_w = None
        self.readers = []


class DSem:
    __slots__ = ("h", "count", "last", "name")

    def __init__(self, h, name=""):
        self.name = name
        self.h = h
        self.count = 0
        self.last = None


class Ins:
    __slots__ = ("eng", "fn", "deps", "sig", "sigval", "dsem", "dval", "is_dma")

    def __init__(self, eng, fn):
        self.eng = eng
        self.fn = fn
        self.deps = []
        self.sig = False
        self.sigval = 0
        self.dsem = None
        self.dval = 0
        self.is_dma = False


class Sched:
    ENGS = ("pe", "act", "dve", "pool", "sp")

    def __init__(self, nc, stack):
        self.nc = nc
        self.stack = stack
        self.eobj = {"pe": nc.tensor, "act": nc.scalar, "dve": nc.vector, "pool": nc.gpsimd, "sp": nc.sync}
        self.esem = {e: stack.enter_context(nc.semaphore("es_" + e)) for e in self.ENGS}
        self.prog = []
        self.last = {e: None for e in self.ENGS}
        self.lastc = {e: None for e in self.ENGS}
        self.bufs = []
        self.dsems = []
        self._dsc = {}

    def buf(self, name):
        b = Buf(name)
        self.bufs.append(b)
        return b

    def bufs_n(self, name, n):
        return [self.buf("%s%d" % (name, i)) for i in range(n)]

    def dsem(self, name):
        name = name.split("%")[0]
        if name in self._dsc:
            return self._dsc[name]
        d = self._dsc[name] = DSem(self.stack.enter_context(self.nc.semaphore("ds_" + name)), name)
        self.dsems.append(d)
        return d

    @staticmethod
    def _need(src, dst, kind):
        if src.is_dma or dst.is_dma:
            return True
        if src.eng != dst.eng:
            return True
        if src.eng == "pe":
            return False
        return True

    def _track(self, ins, r, w):
        deps = ins.deps

        def add(d):
            if d.is_dma:
                deps.append((d.dsem, d.dsem.count))
            else:
                d.sig = True
                deps.append(d)

        for b in r:
            lw = b.last_w
            if lw is not None and self._need(lw, ins, "RAW"):
                add(lw)
        for b in w:
            lw = b.last_w
            if lw is not None and self._need(lw, ins, "WAW"):
                add(lw)
            for rd in b.readers:
                if rd is not ins and self._need(rd, ins, "WAR"):
                    add(rd)
        for b in w:
            b.last_w = ins
            b.readers = []
        for b in r:
            if b.last_w is ins:
                continue
            if not ins.is_dma:
                b.readers = [x for x in b.readers if x.is_dma or x.eng != ins.eng]
            b.readers.append(ins)
        self.prog.append(ins)
        self.last[ins.eng] = ins
        if not ins.is_dma:
            self.lastc[ins.eng] = ins

    def op(self, eng, fn, r=(), w=()):
        ins = Ins(eng, fn)
        self._track(ins, r, w)
        return ins

    def dma(self, eng, out, in_, ds, r=(), w=()):
        ins = Ins(eng, partial(self.eobj[eng].dma_start, out=out, in_=in_))
        if not ds.name.endswith("@" + eng):
            ds = self.dsem(ds.name.split("@")[0] + "@" + eng)
        ins.is_dma = True
        ins.dsem = ds
        self._track(ins, r, w)
        ds.count += 16
        ins.dval = ds.count
        ds.last = ins
        return ins

    def barrier(self):
        lasts = [self.lastc[e] for e in self.ENGS if self.lastc[e] is not None]
        dl = [d.last for d in self.dsems if d.last is not None]
        for e in self.ENGS:
            ins = Ins(e, None)
            for l in lasts:
                if l.eng != e:
                    ins.deps.append(l)
                    l.sig = True
            ins.deps.extend((d.dsem, d.dsem.count) for d in dl)
            self.prog.append(ins)
        for b in self.bufs:
            b.last_w = None
            b.readers = []

    def emit(self):
        cnt = {e: 0 for e in self.ENGS}
        for ins in self.prog:
            if ins.sig and not ins.is_dma and ins.fn is not None:
                cnt[ins.eng] += 1
                ins.sigval = cnt[ins.eng]
        waited = {}
        nwait = 0
        for ins in self.prog:
            E = self.eobj[ins.eng]
            for d in ins.deps:
                if isinstance(d, tuple):
                    sem, val = d[0].h, d[1]
                else:
                    sem, val = self.esem[d.eng], d.sigval
                key = (ins.eng, id(sem))
                if waited.get(key, 0) >= val:
                    continue
                waited[key] = val
                E.wait_ge(sem, val)
                nwait += 1
            if ins.fn is None:
                continue
            bi = ins.fn()
            if ins.is_dma:
                bi.then_inc(ins.dsem.h, 16)
            elif ins.sig:
                bi.then_inc(self.esem[ins.eng], 1)
        return len(self.prog), nwait

    def mm(self, out, lhsT, rhs, start, stop, r, w):
        return self.op("pe", partial(self.nc.tensor.matmul, out, lhsT, rhs, start=start, stop=stop), r, w)

    def tr(self, out, in_, ident, r, w):
        return self.op("pe", partial(self.nc.tensor.transpose, out, in_, ident), r, w)

    def act(self, out, in_, func, r, w, **kw):
        return self.op("act", partial(self.nc.scalar.activation, out, in_, func, **kw), r, w)

    def tt(self, eng, out, in0, in1, op, r, w):
        return self.op(eng, partial(self.eobj[eng].tensor_tensor, out=out, in0=in0, in1=in1, op=op), r, w)

    def ts(self, eng, out, in0, s1, s2, op0, op1, r, w):
        if op1 is None:
            fn = partial(self.eobj[eng].tensor_scalar, out, in0, s1, None, op0)
        else:
            fn = partial(self.eobj[eng].tensor_scalar, out, in0, s1, s2, op0, op1)
        return self.op(eng, fn, r, w)

    def stt(self, eng, out, in0, scalar, in1, op0, op1, r, w, accum_out=None):
        if accum_out is None:
            fn = partial(self.eobj[eng].scalar_tensor_tensor, out, in0, scalar, in1, op0, op1)
        else:
            fn = partial(self.eobj[eng].scalar_tensor_tensor, out, in0, scalar, in1, op0, op1, accum_out)
        return self.op(eng, fn, r, w)

    def cp(self, eng, out, in_, r, w):
        return self.op(eng, partial(self.eobj[eng].tensor_copy, out, in_), r, w)

    def memset(self, eng, ap, val, r, w):
        return self.op(eng, partial(self.eobj[eng].memset, ap, val), r, w)


def _host_consts():
    c = {}
    c["ident"] = np.eye(128, dtype=np.float32)
    p = np.arange(128)[:, None]
    q = np.arange(128)[None, :]
    c["tri"] = np.where(p <= q, 0.0, NEG).astype(np.float32)
    k = np.arange(S)
    n = np.arange(8)
    c["kind"] = (k[None, :] // 256 == n[:, None]).astype(np.float32)
    c["qbias0"] = np.where(n[:, None] <= (k[None, :1024] // 256), 0.0, NEG).astype(np.float32)
    own = (np.arange(8, 16) // 2)
    mn = np.zeros((8, 2, 8), np.float32)
    mm_ = np.zeros((8, 2, 8), np.float32)
    for i in range(8):
        for nn in range(8):
            if nn == own[i]:
                mn[i, :, nn] = 1e30
            elif nn > own[i]:
                mn[i, :, nn] = -1e30
            if nn >= own[i]:
                mm_[i, :, nn] = -1e30
    c["mask_n"] = np.broadcast_to(mn.reshape(1, 128), (128, 128)).copy()
    c["mask_m"] = np.broadcast_to(mm_.reshape(1, 128), (128, 128)).copy()
    fcol = np.zeros((128, 8), np.float32)
    invm = THETA ** (-np.arange(8, dtype=np.float32) * (2.0 / 16))
    invl = THETA ** (-np.arange(16, dtype=np.float32) * (2.0 / 32))
    fcol[:, 0] = 1.0 / (2 * math.pi)
    fcol[:, 1] = 0.0
    for j in range(16):
        fcol[j, 0] = invm[j % 8] / (2 * math.pi)
        fcol[j, 1] = 0.5 if j < 8 else 0.0
    for j in range(32):
        fcol[32 + j, 0] = invl[j % 16] / (2 * math.pi)
        fcol[32 + j, 1] = 0.5 if j < 16 else 0.0
    fcol[:, 2] = 0.25
    fcol[:, 4] = EPS
    c["fcol"] = fcol
    return c


def _host_weights(w_in, w_uq, w_ukv, w_branch_a, w_branch_b, w_out, w_up, w_down, g_pre_mix, b_gate,
                  g_q_norm, g_kv_norm, g_post_mix, g_pre_mlp, g_post_mlp):
    w = {}
    wi = w_in[0]
    cols = []
    for pr in range(4):
        for base0 in (0, 512):
            for h in (2 * pr, 2 * pr + 1):
                b = base0 + h * 64
                cols.extend(range(b, b + 64))
                cols.extend(range(b + 8, b + 16))
                cols.extend(range(b, b + 8))
    w["w_qk"] = np.ascontiguousarray(wi[:, cols])
    w["w_v"] = np.ascontiguousarray(wi[:, 1024:1536])
    w["w_cq"] = np.ascontiguousarray(wi[:, 1536:1920])
    w["w_ckv"] = np.ascontiguousarray(wi[:, 1920:2176])
    kr = list(range(2176, 2208)) + list(range(2176 + 16, 2208)) + list(range(2176, 2176 + 16))
    w["w_kr"] = np.ascontiguousarray(wi[:, kr])
    w["w_gate"] = np.ascontiguousarray(wi[:, 2208:4256])
    uq = w_uq[0]
    cols = []
    for h in range(8):
        b = h * 96
        cols.extend(range(b + 64, b + 96))
        cols.extend(range(b + 80, b + 96))
        cols.extend(range(b + 64, b + 80))
        cols.extend(range(b, b + 64))
    w["w_uq"] = np.ascontiguousarray(uq[:, cols])
    ukv = w_ukv[0]
    wk = np.zeros((256, 8, 128), np.float32)
    wv = np.zeros((256, 8, 64), np.float32)
    for h in range(8):
        wk[:, h, 64:128] = ukv[:, h * 128:h * 128 + 64]
        wv[:, h, :] = ukv[:, h * 128 + 64:h * 128 + 128]
    w["w_ukvk"] = wk.reshape(256, 1024)
    w["w_ukvv"] = wv.reshape(256, 512)
    w["w_a"] = np.ascontiguousarray(w_branch_a[0])
    w["w_b"] = np.ascontiguousarray(w_branch_b[0])
    w["w_out"] = np.ascontiguousarray(w_out[0])
    w["w_up"] = np.ascontiguousarray(w_up[0])
    w["w_down"] = np.ascontiguousarray(w_down[0])

    def colsT(v, nk):
        return np.ascontiguousarray(v.reshape(nk, 128).T)

    w["gpre_T"] = colsT(g_pre_mix[0], 8)
    w["gpre2_T"] = colsT(g_pre_mlp[0], 8)
    w["gq_T"] = colsT(g_q_norm[0], 3)
    w["gkv_T"] = colsT(g_kv_norm[0], 2)
    w["bgate_T"] = colsT(b_gate[0], 16)
    w["gpm"] = np.ascontiguousarray(g_post_mix[0].reshape(1, 1024))
    w["gpl"] = np.ascontiguousarray(g_post_mlp[0].reshape(1, 1024))
    return {k: np.asarray(v, dtype=np.float32) for k, v in w.items()}


def build_nc(nseq=NSEQ, dbg=None):
    try:
        return _build_nc(nseq, dbg)
    except _Stop:
        return _STOP[2]


def _build_nc(nseq=NSEQ, dbg=None):
    nc = bass.Bass("TRN2", target_bir_lowering=False)
    _STOP[2] = nc
    NTOK = nseq * S

    def din(name, shape, dt=F32):
        return nc.dram_tensor(name, list(shape), dt, kind="ExternalInput").ap()

    x_d = din("x", [NTOK, D])
    pos_d = din("pos", [nseq, S], I32)
    w_qk_d = din("w_qk", [1024, 1280]).rearrange("(kc p) n -> p kc n", p=128)
    w_v_d = din("w_v", [1024, 512]).rearrange("(kc p) n -> p kc n", p=128)
    w_cq_d = din("w_cq", [1024, 384]).rearrange("(kc p) n -> p kc n", p=128)
    w_ckv_d = din("w_ckv", [1024, 256]).rearrange("(kc p) n -> p kc n", p=128)
    w_kr_d = din("w_kr", [1024, 64]).rearrange("(kc p) n -> p kc n", p=128)
    w_gate_d = din("w_gate", [1024, 2048]).rearrange("(kc p) n -> p kc n", p=128)
    w_uq_d = din("w_uq", [384, 1024]).rearrange("(kc p) n -> p kc n", p=128)
    w_ukvk_d = din("w_ukvk", [256, 1024]).rearrange("(kc p) n -> p kc n", p=128)
    w_ukvv_d = din("w_ukvv", [256, 512]).rearrange("(kc p) n -> p kc n", p=128)
    w_a_d = din("w_a", [512, 1024]).rearrange("(kc p) n -> p kc n", p=128)
    w_b_d = din("w_b", [512, 1024]).rearrange("(kc p) n -> p kc n", p=128)
    w_out_d = din("w_out", [1024, 1024]).rearrange("(kc p) n -> p kc n", p=128)
    w_up_d = din("w_up", [1024, 4096]).rearrange("(kc p) n -> p kc n", p=128)
    w_down_d = din("w_down", [4096, 1024]).rearrange("(kc p) n -> p kc n", p=128)
    gpre_d = din("gpre_T", [128, 8])
    gpre2_d = din("gpre2_T", [128, 8])
    gq_d = din("gq_T", [128, 3])
    gkv_d = din("gkv_T", [128, 2])
    bgate_d = din("bgate_T", [128, 16])
    gpm_d = din("gpm", [1, 1024])
    gpl_d = din("gpl", [1, 1024])
    ident_d = din("ident", [128, 128])
    tri_d = din("tri", [128, 128])
    kind_d = din("kind", [8, S])
    qbias0_d = din("qbias0", [8, 1024])
    maskn_d = din("mask_n", [128, 128])
    maskm_d = din("mask_m", [128, 128])
    fcol_d = din("fcol", [128, 8])
    out_d = nc.dram_tensor("out", [NTOK, D], F32, kind="ExternalOutput").ap()
    dbg_d = {}
    if dbg:
        for name, shape in dbg.items():
            dbg_d[name] = nc.dram_tensor("dbg_" + name, list(shape), BF16, kind="ExternalOutput").ap()

    with ExitStack() as st:
        sc = Sched(nc, st)
        _STOP[1] = sc

        uniq = [0]

        def sb(name, shape, dt, stack=st):
            uniq[0] += 1
            return stack.enter_context(nc.sbuf_tensor("s%d_%s" % (uniq[0], name), list(shape), dt))

        ps = st.enter_context(nc.psum_tensor("psum_all", [128, 8, 512], F32))
        psb = ps.bitcast(BF16) if hasattr(ps, "bitcast") else None
        B_ps = sc.bufs_n("ps", 8)

        ident = sb("ident", [128, 128], BF16)
        tri = sb("tri", [128, 128], BF16)
        gpre = sb("gpre", [128, 8], F32)
        gpre2 = sb("gpre2", [128, 8], F32)
        gq = sb("gq", [128, 3], F32)
        gkv = sb("gkv", [128, 2], F32)
        bgate = sb("bgate", [128, 16], F32)
        fcol = sb("fcol", [128, 8], F32)
        ones_bf = sb("ones_bf", [128, 128], BF16)
        hT = sb("hT", [128, 8, S], BF16)
        oTa = sb("oTa", [128, 4, S], BF16)
        oTb = sb("oTb", [128, 4, S], BF16)
        B_const = sc.buf("const")
        B_hT = sc.bufs_n("hT", 4)
        B_oTa = [sc.bufs_n("oTa%d_" % p, 4) for p in range(4)]
        B_oTb = [sc.bufs_n("oTb%d_" % p, 4) for p in range(4)]
        ds_const = sc.dsem("const")

        for (t, d) in ((gpre, gpre_d), (gpre2, gpre2_d), (gq, gq_d), (gkv, gkv_d), (bgate, bgate_d),
                       (fcol, fcol_d)):
            sc.dma("sp", t[:], d[:, :], ds_const, w=[B_const])
        sc.dma("pool", ident[:], ident_d[:, :], ds_const, w=[B_const])
        sc.dma("pool", tri[:], tri_d[:, :], ds_const, w=[B_const])
        sc.memset("dve", ones_bf[:], 1.0, r=[], w=[B_const])
        sc.barrier()

        xt_sem = [sc.dsem("xt0"), sc.dsem("xt1")]
        out_sem = [sc.dsem("o0"), sc.dsem("o1")]
        B_x1d = [sc.bufs_n("x1d%d_" % s, 16) for s in range(nseq)]

        def norm_transpose(src_tile, B_src, tt_col, xn, B_xn, ssq, rstd, B_st, junk, B_junk, bank, gcol, dstT,
                           B_dst):
            sc.stt("dve", junk[:], src_tile, 1.0, src_tile, ALU.mult, ALU.mult, r=[B_src], w=[B_junk, B_st],
                   accum_out=ssq[:, 0:1])
            sc.act(rstd[:, 0:1], ssq[:, 0:1], AF.Sqrt, r=[B_st, B_const], w=[B_st], bias=fcol[:, 4:5], scale=1.0 / D)
            sc.op("dve", partial(nc.vector.reciprocal, rstd[:, 0:1], rstd[:, 0:1]), r=[B_st], w=[B_st])
            sc.ts("dve", xn[:], src_tile, rstd[:, 0:1], None, ALU.mult, None, r=[B_src, B_st], w=[B_xn])
            pT = psb[:, bank, :]
            for kc in range(8):
                sc.tr(pT[:, kc * 128:(kc + 1) * 128], xn[:, kc * 128:(kc + 1) * 128], ident[:], r=[B_xn, B_const],
                      w=[B_ps[bank]])
            sc.tt("dve", dstT[:, :, tt_col:tt_col + 128], pT.rearrange("p (k t) -> p k t", k=8),
                  gcol[:, :].unsqueeze(2).broadcast_to([128, 8, 128]), ALU.mult, r=[B_ps[bank], B_const],
                  w=[B_dst])

        try:
          for s in range(nseq):
              r0 = s * S
              with ExitStack() as p2:
                  def sb2(name, shape, dt):
                      return sb(name, shape, dt, p2)

                  COS = sb2("COS", [128, S], F32)
                  SIN = sb2("SIN", [128, S], F32)
                  p1 = p2.enter_context(ExitStack())

                  def sb1(name, shape, dt):
                      return sb(name, shape, dt, p1)

                  xt = [sb1("xt0", [128, 1024], F32), sb1("xt1", [128, 1024], F32)]
                  xn = [sb1("xn0", [128, 1024], BF16), sb1("xn1", [128, 1024], BF16)]
                  junk = sb1("junk", [128, 1024], F32)
                  ssq = sb1("ssq", [128, 2], F32)
                  rstd = sb1("rstd", [128, 2], F32)
                  B_xt = sc.bufs_n("xt", 2)
                  B_xn = sc.bufs_n("xn", 2)
                  B_junk = sc.buf("junk")
                  B_st = sc.bufs_n("st", 2)
                  posi = sb1("posi", [128, S], I32)
                  B_tab = sc.buf("tab")
                  B_posi = sc.buf("posi")
                  ds_pos = sc.dsem("pos")

                  sc.dma("sp", posi[:], pos_d[s:s + 1, :].partition_broadcast(128), ds_pos, w=[B_posi])
                  kint = sb1("kint", [128, 1024], I32)
                  kflt = sb1("kflt", [128, 1024], F32)
                  B_kint = sc.buf("kint")
                  for hh in range(2):
                      cs = slice(hh * 1024, (hh + 1) * 1024)
                      sc.cp("dve", junk[:, :], posi[:, cs], r=[B_posi], w=[B_junk])
                      for (tab, phc) in ((SIN, 1), (COS, 2)):
                          sc.ts("dve", tab[:, cs], junk[:, :], fcol[:, 0:1], fcol[:, phc:phc + 1], ALU.mult, ALU.add,
                                r=[B_junk, B_const], w=[B_tab])
                          sc.cp("dve", kint[:, :], tab[:, cs], r=[B_tab], w=[B_kint])
                          sc.cp("dve", kflt[:, :], kint[:, :], r=[B_kint], w=[B_kint])
                          sc.tt("dve", tab[:, cs], tab[:, cs], kflt[:, :], ALU.subtract, r=[B_tab, B_kint], w=[B_tab])
                          sc.act(tab[:, cs], tab[:, cs], AF.Sin, r=[B_tab], w=[B_tab], scale=6.2831845)

                  _ck("tables")
                  for tt in range(16):
                      sl = tt % 2
                      sc.dma("sp", xt[sl][:], x_d[r0 + tt * 128:r0 + (tt + 1) * 128, :], xt_sem[sl], w=[B_xt[sl]])
                      norm_transpose(xt[sl][:], B_xt[sl], tt * 128, xn[sl], B_xn[sl], ssq[:, sl:sl + 1],
                                     rstd[:, sl:sl + 1], B_st[sl], junk, B_junk, sl, gpre, hT, B_hT[tt // 4])

                  sc.barrier()
                  p1.close()

                  _ck("phase1")
                  QT = [sb2("QT0", [128, S], BF16), sb2("QT1", [128, S], BF16)]
                  KT = [sb2("KT0", [128, S], BF16), sb2("KT1", [128, S], BF16)]
                  B_QT = [sc.bufs_n("QT%d_" % i, 4) for i in range(2)]
                  B_KT = [sc.bufs_n("KT%d_" % i, 4) for i in range(2)]
                  B_QTx = sc.bufs_n("QTx", 2)
                  B_KTx = sc.bufs_n("KTx", 2)
                  Vaug = sb2("Vaug", [128, 16, 4, 192], BF16)
                  B_V = sc.bufs_n("V", 16)
                  B_Vones = sc.buf("Vones")
                  PT = [sb2("PT%d" % i, [128, 512], BF16) for i in range(3)]
                  B_PT = sc.bufs_n("PT", 3)
                  rtmp = [sb2("rtmp%d" % i, [128, 512], F32) for i in range(2)]
                  B_rtmp = sc.bufs_n("rtmp", 2)
                  t1 = sb2("t1", [128, 512], F32)
                  t2 = sb2("t2", [128, 512], F32)
                  B_t1 = sc.buf("t1")
                  B_t2 = sc.buf("t2")
                  B_wv = sc.buf("wv")
                  ds_wv = sc.dsem("wv")
                  pm = p2.enter_context(ExitStack())

                  def sb2m(name, shape, dt):
                      return sb(name, shape, dt, pm)

                  wv = sb2m("wv", [128, 8, 512], BF16)

                  wqk = [sb2m("wqk0", [128, 8, 320], BF16), sb2m("wqk1", [128, 8, 320], BF16)]
                  B_wqk = sc.bufs_n("wqk", 2)
                  ds_wqk = [sc.dsem("wqk0"), sc.dsem("wqk1")]
                  kmT = sb2m("kmT", [128, 2, 8], F32)
                  kmTb = sb2m("kmTb", [128, 2, 8], BF16)
                  B_km = sc.bufs_n("km", 2)
                  gn = sb2m("gn", [128, 128], F32)
                  gm = sb2m("gm", [128, 128], F32)
                  rk = sb2m("rk", [128, 128], F32)
                  cmpt = sb2m("cmpt", [128, 128], F32)
                  maskn = sb2m("maskn", [128, 128], F32)
                  maskm = sb2m("maskm", [128, 128], F32)
                  bpad = sb2m("bpad", [128, 8, 2, 72], BF16)
                  B_g = sc.buf("gating")
                  B_bpad = sc.buf("bpad")
                  ds_misc = sc.dsem("misc")
                  sc.dma("sp", maskn[:], maskn_d[:, :], ds_misc, w=[B_g])
                  sc.dma("sp", maskm[:], maskm_d[:, :], ds_misc, w=[B_g])
                  sc.memset("pool", bpad[:], 0.0, r=[], w=[B_bpad])
                  sc.memset("pool", Vaug[:, :, :, 64:128], 1.0, r=[], w=[B_Vones])

                  state = {"sb": 0, "pt": 0, "acc": 0, "pend": None, "rt": 0}

                  def flush_pv():
                      pd = state["pend"]
                      if pd is None:
                          return
                      state["pend"] = None
                      (hl, pr, kt, c0, ptb, acc, first, last, qi, oT, B_oT) = pd
                      sc.mm(ps[:, acc, c0:512], Vaug[:, kt, pr, hl * 64:hl * 64 + 128], PT[ptb][:, c0:512],
                            first, last, r=[B_V[kt], B_Vones, B_PT[ptb]], w=[B_ps[acc]])
                      if last:
                          ri = state["rt"]
                          state["rt"] = 1 - ri
                          o_lo, d_lo = (0, 64) if hl == 0 else (64, 0)
                          sc.op("dve", partial(nc.vector.reciprocal, rtmp[ri][o_lo:o_lo + 64, :],
                                               ps[d_lo:d_lo + 64, acc, :]), r=[B_ps[acc]], w=[B_rtmp[ri]])
                          sc.tt("dve", oT[o_lo:o_lo + 64, pr, qi * 512:(qi + 1) * 512], ps[o_lo:o_lo + 64, acc, :],
                                rtmp[ri][o_lo:o_lo + 64, :], ALU.mult, r=[B_ps[acc], B_rtmp[ri]], w=[B_oT[pr][qi]])

                  def attention(hl, pr, R, scale, oT, B_oT):
                      for qi in range(4):
                          q0 = qi * 512
                          nkt = 4 * qi + 4
                          acc = 5 + state["acc"]
                          state["acc"] = 1 - state["acc"]
                          for kt in range(nkt):
                              rdiag = kt - 4 * qi
                              c0 = 128 * rdiag if rdiag > 0 else 0
                              sbk = 2 + state["sb"]
                              state["sb"] = (state["sb"] + 1) % 3
                              ptb = state["pt"]
                              state["pt"] = (state["pt"] + 1) % 3
                              sc.mm(ps[:, sbk, c0:512], KT[hl][0:R, kt * 128:(kt + 1) * 128],
                                    QT[hl][0:R, q0 + c0:q0 + 512], True, rdiag < 0,
                                    r=[B_KT[hl][kt // 4], B_KTx[hl], B_QT[hl][qi], B_QTx[hl]], w=[B_ps[sbk]])
                              if rdiag >= 0:
                                  sc.mm(ps[:, sbk, c0:c0 + 128], ident[:], tri[:], False, True, r=[B_const],
                                        w=[B_ps[sbk]])
                              sc.act(PT[ptb][:, c0:512], ps[:, sbk, c0:512], AF.Exp, r=[B_ps[sbk]], w=[B_PT[ptb]],
                                     scale=scale)
                              flush_pv()
                              state["pend"] = (hl, pr, kt, c0, ptb, acc, kt == 0, kt == nkt - 1, qi, oT, B_oT)

                  def rope_rows(dst, nrow, pst, swap_lo, cos_lo, g, B_dst):
                      cs = slice(g * 512, (g + 1) * 512)
                      sc.tt("dve", t1[0:nrow, :], pst[swap_lo:swap_lo + nrow, :], SIN[cos_lo:cos_lo + nrow, cs],
                            ALU.mult, r=[B_ps_cur[0], B_tab, B_dst], w=[B_t1])
                      sc.tt("dve", t2[0:nrow, :], pst[0:nrow, :], COS[cos_lo:cos_lo + nrow, cs], ALU.mult,
                            r=[B_ps_cur[0], B_tab, B_dst], w=[B_t2])
                      sc.tt("dve", dst[0:nrow, cs], t1[0:nrow, :], t2[0:nrow, :], ALU.add, r=[B_t1, B_t2],
                            w=[B_dst])

                  B_ps_cur = [None]
                  pbank = {"i": 0}

                  def next_pbank():
                      b = pbank["i"]
                      pbank["i"] = 1 - b
                      B_ps_cur[0] = B_ps[b]
                      return b

                  sc.dma("pool", wv[:], w_v_d[:, :, :], ds_wv, w=[B_wv])
                  sc.dma("pool", wqk[0][:], w_qk_d[:, :, 0:320], ds_wqk[0], w=[B_wqk[0]])
                  _ck("p2init")
                  for tt in range(16):
                      b = next_pbank()
                      for kc in range(8):
                          sc.mm(ps[:, b, :], hT[:, kc, tt * 128:(tt + 1) * 128], wv[:, kc, :], kc == 0, kc == 7,
                                r=[B_hT[tt // 4], B_wv], w=[B_ps[b]])
                      src = ps[:, b, :].rearrange("p (a e d) -> p a e d", a=4, e=2)
                      sc.act(Vaug[:, tt, :, 0:64], src[:, :, 0, :], AF.Copy, r=[B_ps[b]], w=[B_V[tt]])
                      sc.cp("dve", Vaug[:, tt, :, 128:192], src[:, :, 1, :], r=[B_ps[b]], w=[B_V[tt]])
                  for pr in range(4):
                      wsl = pr % 2
                      if pr + 1 < 4:
                          sc.dma("pool", wqk[1 - wsl][:], w_qk_d[:, :, (pr + 1) * 320:(pr + 2) * 320], ds_wqk[1 - wsl],
                                 w=[B_wqk[1 - wsl]])
                      for hl in (range(2) if pr == 0 else ()):
                          sc.dma("pool", KT[hl][64:72, :], kind_d[:, :], ds_misc, w=[B_KTx[hl]])
                          sc.dma("pool", QT[hl][64:72, 0:1024], qbias0_d[:, :], ds_misc, w=[B_QTx[hl]])
                      _ck("mobaV")
                      for j in range(4):
                          hl = j % 2
                          dst = QT[hl] if j < 2 else KT[hl]
                          B_dst = B_QT[hl] if j < 2 else B_KT[hl]
                          for g in range(4):
                              b = next_pbank()
                              for kc in range(8):
                                  sc.mm(ps[0:80, b, :], wqk[wsl][:, kc, j * 80:(j + 1) * 80],
                                        hT[:, kc, g * 512:(g + 1) * 512], kc == 0, kc == 7,
                                        r=[B_wqk[wsl], B_hT[g]], w=[B_ps[b]])
                              sc.act(dst[0:64, g * 512:(g + 1) * 512], ps[0:64, b, :], AF.Copy, r=[B_ps[b]],
                                     w=[B_dst[g]])
                              rope_rows(dst, 16, ps[:, b, :], 64, 0, g, B_dst[g])
                      _ck("mobaproj")
                      for hl in range(2):
                          sc.op("dve", partial(nc.vector.tensor_reduce, kmT[0:64, hl, :],
                                               KT[hl][0:64, :].rearrange("p (n k) -> p n k", n=8), AX.X, ALU.add),
                                r=B_KT[hl], w=[B_km[hl]])
                          sc.ts("dve", kmTb[0:64, hl, :], kmT[0:64, hl, :], 1.0 / 256, None, ALU.mult, None,
                                r=[B_km[hl]], w=[B_km[hl]])
                      gb = 7
                      for qt in range(8):
                          for hl in range(2):
                              sc.mm(ps[:, gb, qt * 16 + hl * 8:qt * 16 + hl * 8 + 8],
                                    QT[hl][0:64, (8 + qt) * 128:(9 + qt) * 128], kmTb[0:64, hl, :], True, True,
                                    r=[B_QT[hl][2 + qt // 4], B_km[hl]], w=[B_ps[gb]])
                      sc.tt("dve", gn[:], ps[:, gb, 0:128], maskn[:], ALU.add, r=[B_ps[gb], B_g], w=[B_g])
                      sc.tt("dve", gm[:], ps[:, gb, 0:128], maskm[:], ALU.add, r=[B_ps[gb], B_g], w=[B_g])
                      gn3 = gn[:].rearrange("p (a n) -> p a n", n=8)
                      gm3 = gm[:].rearrange("p (a n) -> p a n", n=8)
                      rk3 = rk[:].rearrange("p (a n) -> p a n", n=8)
                      cm3 = cmpt[:].rearrange("p (a n) -> p a n", n=8)
                      for m in range(7):
                          dst3 = rk3 if m == 0 else cm3
                          sc.tt("dve", dst3, gm3[:, :, m:m + 1].broadcast_to([128, 16, 8]), gn3, ALU.is_gt, r=[B_g],
                                w=[B_g])
                          if m > 0:
                              sc.tt("dve", rk[:], rk[:], cmpt[:], ALU.add, r=[B_g], w=[B_g])
                      sc.ts("dve", bpad[:, :, :, 64:72], rk[:].rearrange("p (q h n) -> p q h n", q=8, h=2), 3.0, NEG,
                            ALU.is_ge, ALU.mult, r=[B_g], w=[B_bpad])
                      for hl in range(2):
                          for gg in range(2):
                              for q4 in range(4):
                                  qt = gg * 4 + q4
                                  sc.mm(ps[0:72, gb, q4 * 128:(q4 + 1) * 128], bpad[:, qt, hl, :], ident[:], True, True,
                                        r=[B_bpad, B_const], w=[B_ps[gb]])
                              sc.cp("dve", QT[hl][64:72, 1024 + gg * 512:1024 + (gg + 1) * 512], ps[64:72, gb, :],
                                    r=[B_ps[gb]], w=[B_QTx[hl]])
                      _ck("mobagate")
                      for hl in range(2):
                          attention(hl, pr, 72, 0.125, oTa, B_oTa)
                      flush_pv()

                  sc.barrier()
                  pm.close()
                  _ck("moba")
                  cqT = sb2("cqT", [128, 3, S], BF16)
                  ckvT = sb2("ckvT", [128, 2, S], BF16)
                  kropeT = sb2("kropeT", [32, S], BF16)
                  sqt = [sb2("sqt0", [128, 512], BF16), sb2("sqt1", [128, 512], BF16)]
                  rbc = sb2("rbc", [128, 512], F32)
                  B_cq = sc.bufs_n("cq", 4)
                  B_ckv = sc.bufs_n("ckv", 4)
                  B_krope = sc.bufs_n("krope", 4)
                  B_sqt = sc.bufs_n("sqt", 2)
                  B_rbc = sc.buf("rbc")
                  wc = sb2("wc", [128, 8, 384], BF16)
                  B_wc = sc.buf("wc")
                  ds_wc = sc.dsem("wc")
                  wuq = sb2("wuq", [128, 3, 256], BF16)
                  wukk = sb2("wukk", [128, 2, 256], BF16)
                  wvb = sb2("wvb", [128, 2, 512], BF16)
                  B_wu = sc.buf("wu")
                  ds_wu = sc.dsem("wu")
                  sc.dma("pool", wc[:, :, 0:384], w_cq_d[:, :, :], ds_wc, w=[B_wc])
                  sc.dma("pool", wvb[:], w_ukvv_d[:, :, :], ds_wv, w=[B_wv])

                  def latent(nch, col0, gcol, dstT, B_dst, g, nfeat):
                      cs = slice(g * 512, (g + 1) * 512)
                      sbank = 7
                      for c in range(nch):
                          b = c
                          for kc in range(8):
                              sc.mm(ps[:, b, :], wc[:, kc, col0 + c * 128:col0 + (c + 1) * 128], hT[:, kc, cs],
                                    kc == 0, kc == 7, r=[B_wc, B_hT[g]], w=[B_ps[b]])
                          sq = sqt[c % 2]
                          sc.act(sq[:], ps[:, b, :], AF.Square, r=[B_ps[b]], w=[B_sqt[c % 2]])
                          sc.mm(ps[:, sbank, :], ones_bf[:], sq[:], c == 0, c == nch - 1, r=[B_const, B_sqt[c % 2]],
                                w=[B_ps[sbank]])
                      sc.act(rbc[:], ps[:, sbank, :], AF.Sqrt, r=[B_ps[sbank], B_const], w=[B_rbc], bias=fcol[:, 4:5],
                             scale=1.0 / nfeat)
                      sc.op("dve", partial(nc.vector.reciprocal, rbc[:], rbc[:]), r=[B_rbc], w=[B_rbc])
                      for c in range(nch):
                          sc.stt("dve", dstT[:, c, cs], ps[:, c, :], gcol[:, c:c + 1], rbc[:], ALU.mult, ALU.mult,
                                 r=[B_ps[c], B_rbc, B_const], w=[B_dst[g]])

                  for g in range(4):
                      latent(3, 0, gq, cqT, B_cq, g, 384.0)
                  sc.dma("pool", wc[:, :, 0:256], w_ckv_d[:, :, :], ds_wc, w=[B_wc])
                  for g in range(4):
                      latent(2, 0, gkv, ckvT, B_ckv, g, 256.0)
                  sc.dma("pool", wc[:, :, 0:64], w_kr_d[:, :, :], ds_wc, w=[B_wc])
                  for g in range(4):
                      b = next_pbank()
                      for kc in range(8):
                          sc.mm(ps[0:64, b, :], wc[:, kc, 0:64], hT[:, kc, g * 512:(g + 1) * 512], kc == 0, kc == 7,
                                r=[B_wc, B_hT[g]], w=[B_ps[b]])
                      rope_rows(kropeT, 32, ps[:, b, :], 32, 32, g, B_krope[g])
                  _ck("mlaprep")
                  for tt in range(16):
                      b = next_pbank()
                      for kc in range(2):
                          sc.mm(ps[:, b, :], ckvT[:, kc, tt * 128:(tt + 1) * 128], wvb[:, kc, :], kc == 0, kc == 1,
                                r=[B_ckv[tt // 4], B_wv], w=[B_ps[b]])
                      src = ps[:, b, :].rearrange("p (a e d) -> p a e d", a=4, e=2)
                      sc.act(Vaug[:, tt, :, 0:64], src[:, :, 0, :], AF.Copy, r=[B_ps[b]], w=[B_V[tt]])
                      sc.cp("dve", Vaug[:, tt, :, 128:192], src[:, :, 1, :], r=[B_ps[b]], w=[B_V[tt]])
                  _ck("mlaV")
                  for pr in range(4):
                      sc.dma("pool", wuq[:], w_uq_d[:, :, pr * 256:(pr + 1) * 256], ds_wu, w=[B_wu])
                      sc.dma("pool", wukk[:], w_ukvk_d[:, :, pr * 256:(pr + 1) * 256], ds_wu, w=[B_wu])
                      _ck("mp1")
                      for hl in range(2):
                          h = 2 * pr + hl
                          for g in range(4):
                              cs = slice(g * 512, (g + 1) * 512)
                              b = next_pbank()
                              for kc in range(3):
                                  sc.mm(ps[:, b, :], wuq[:, kc, hl * 128:(hl + 1) * 128], cqT[:, kc, cs], kc == 0, kc == 2,
                                        r=[B_wu, B_cq[g]], w=[B_ps[b]])
                              _ck("mp2a")
                              sc.act(QT[hl][:, cs], ps[:, b, :], AF.Copy, r=[B_ps[b]], w=[B_QT[hl][g], B_QTx[hl]])
                              _ck("mp2")
                              rope_rows(QT[hl], 32, ps[:, b, :], 32, 32, g, B_QT[hl][g])
                              _ck("mp3")
                              b = next_pbank()
                              for kc in range(2):
                                  sc.mm(ps[:, b, :], wukk[:, kc, hl * 128:(hl + 1) * 128], ckvT[:, kc, cs], kc == 0,
                                        kc == 1, r=[B_wu, B_ckv[g]], w=[B_ps[b]])
                              sc.act(KT[hl][:, cs], ps[:, b, :], AF.Copy, r=[B_ps[b]], w=[B_KT[hl][g], B_KTx[hl]])
                              _ck("mp4")
                              sc.cp("dve", KT[hl][0:32, cs], kropeT[:, cs], r=[B_krope[g]], w=[B_KT[hl][g]])
                              _ck("mp5")
                      _ck("mlaproj")
                      for hl in range(2):
                          attention(hl, pr, 128, 96.0 ** -0.5, oTb, B_oTb)
                      flush_pv()
                  sc.barrier()
                  if dbg and s == 0:
                      for name in dbg:
                          src = {"hT": hT, "oTa": oTa, "oTb": oTb}[name]
                          for kc in range(dbg[name][0] // 128):
                              sc.dma("sp", dbg_d[name][kc * 128:(kc + 1) * 128, :], src[:, kc, :], out_sem[0], w=[])
                  sc.barrier()

              _ck("phase2")
              with ExitStack() as p3:
                  def sb3(name, shape, dt):
                      return sb(name, shape, dt, p3)

                  mergedT = sb3("mergedT", [128, 8, S], BF16)
                  gpm_bc = sb3("gpm_bc", [128, 1024], F32)
                  B_gbc = sc.buf("gbc")
                  sc.dma("sp", gpm_bc[:], gpm_d[0:1, :].partition_broadcast(128), sc.dsem("gbc"), w=[B_gbc])
                  B_mg = sc.bufs_n("mg", 4)
                  wo = sb3("wo", [128, 8, 1024], BF16)
                  B_wo = sc.buf("wo")
                  ds_wo = sc.dsem("wo")
                  wg = [sb3("wg0", [128, 8, 256], BF16), sb3("wg1", [128, 8, 256], BF16)]
                  wab = [sb3("wab0", [128, 4, 256], BF16), sb3("wab1", [128, 4, 256], BF16)]
                  B_wg = sc.bufs_n("wg", 2)
                  ds_wg = [sc.dsem("wg0"), sc.dsem("wg1")]
                  sig = [[sb3("sig%d%d" % (a, b), [128, 512], F32) for b in range(2)] for a in range(2)]
                  mt = [[sb3("mt%d%d" % (a, b), [128, 512], F32) for b in range(2)] for a in range(2)]
                  B_sig = [sc.bufs_n("sig%d_" % a, 2) for a in range(2)]
                  B_mt = [sc.bufs_n("mt%d_" % a, 2) for a in range(2)]
                  ysb = sb3("ysb", [128, 1024], F32)
                  tmpy = sb3("tmpy", [128, 1024], F32)
                  junk3 = sb3("junk3", [128, 1024], F32)
                  xt3 = [sb3("xt3_0", [128, 1024], F32), sb3("xt3_1", [128, 1024], F32)]
                  x1t = [sb3("x1t0", [128, 1024], F32), sb3("x1t1", [128, 1024], F32)]
                  st3 = sb3("st3", [128, 2], F32)
                  B_ysb = sc.buf("ysb")
                  B_tmpy = sc.buf("tmpy")
                  B_junk3 = sc.buf("junk3")
                  B_xt3 = sc.bufs_n("xt3", 2)
                  B_x1t = sc.bufs_n("x1t", 2)
                  B_st3 = sc.buf("st3")

                  def load_w3(fo, sl):
                      sc.dma("pool", wg[sl][:, :, 0:128], w_gate_d[:, :, fo * 128:(fo + 1) * 128], ds_wg[sl],
                             w=[B_wg[sl]])
                      sc.dma("pool", wg[sl][:, :, 128:256], w_gate_d[:, :, 1024 + fo * 128:1024 + (fo + 1) * 128],
                             ds_wg[sl], w=[B_wg[sl]])
                      sc.dma("pool", wab[sl][:, :, 0:128], w_a_d[:, :, fo * 128:(fo + 1) * 128], ds_wg[sl],
                             w=[B_wg[sl]])
                      sc.dma("pool", wab[sl][:, :, 128:256], w_b_d[:, :, fo * 128:(fo + 1) * 128], ds_wg[sl],
                             w=[B_wg[sl]])

                  load_w3(0, 0)
                  sc.dma("pool", wo[:], w_out_d[:, :, :], ds_wo, w=[B_wo])
                  it = 0
                  for fo in range(8):
                      sl = fo % 2
                      if fo + 1 < 8:
                          load_w3(fo + 1, 1 - sl)
                      for g in range(4):
                          cs = slice(g * 512, (g + 1) * 512)
                          a = it % 2
                          it += 1
                          bb = 4 * a
                          for half in range(2):
                              for kc in range(8):
                                  sc.mm(ps[:, bb + half, :], wg[sl][:, kc, half * 128:(half + 1) * 128], hT[:, kc, cs],
                                        kc == 0, kc == 7, r=[B_wg[sl], B_hT[g]], w=[B_ps[bb + half]])
                              sc.act(sig[a][half][:], ps[:, bb + half, :], AF.Sigmoid, r=[B_ps[bb + half], B_const],
                                     w=[B_sig[a][half]], bias=bgate[:, half * 8 + fo:half * 8 + fo + 1], scale=1.0)
                          for half in range(2):
                              oT = oTa if half == 0 else oTb
                              B_oT = B_oTa if half == 0 else B_oTb
                              for pr in range(4):
                                  sc.mm(ps[:, bb + 2 + half, :], wab[sl][:, pr, half * 128:(half + 1) * 128],
                                        oT[:, pr, cs], pr == 0, pr == 3, r=[B_wg[sl], B_oT[pr][g]],
                                        w=[B_ps[bb + 2 + half]])
                              sc.tt("dve", mt[a][half][:], ps[:, bb + 2 + half, :], sig[a][half][:], ALU.mult,
                                    r=[B_ps[bb + 2 + half], B_sig[a][half]], w=[B_mt[a][half]])
                          sc.tt("pool", mergedT[:, fo, cs], mt[a][0][:], mt[a][1][:], ALU.add,
                                r=[B_mt[a][0], B_mt[a][1]], w=[B_mg[g]])
                  for tt in range(16):
                      sl = tt % 2
                      yb = 2 * sl
                      sc.dma("sp", xt3[sl][:], x_d[r0 + tt * 128:r0 + (tt + 1) * 128, :], xt_sem[sl], w=[B_xt3[sl]])
                      for half in range(2):
                          for fo in range(8):
                              sc.mm(ps[:, yb + half, :], mergedT[:, fo, tt * 128:(tt + 1) * 128],
                                    wo[:, fo, half * 512:(half + 1) * 512], fo == 0, fo == 7, r=[B_mg[tt // 4], B_wo],
                                    w=[B_ps[yb + half]])
                      sc.act(ysb[:, 0:512], ps[:, yb, :], AF.Copy, r=[B_ps[yb]], w=[B_ysb])
                      sc.act(ysb[:, 512:1024], ps[:, yb + 1, :], AF.Copy, r=[B_ps[yb + 1]], w=[B_ysb])
                      sc.stt("dve", junk3[:], ysb[:], 1.0, ysb[:], ALU.mult, ALU.mult, r=[B_ysb], w=[B_junk3, B_st3],
                             accum_out=st3[:, 0:1])
                      sc.act(st3[:, 1:2], st3[:, 0:1], AF.Sqrt, r=[B_st3, B_const], w=[B_st3], bias=fcol[:, 4:5], scale=1.0 / D)
                      sc.op("dve", partial(nc.vector.reciprocal, st3[:, 1:2], st3[:, 1:2]), r=[B_st3], w=[B_st3])
                      sc.stt("dve", tmpy[:], ysb[:], st3[:, 1:2], gpm_bc[:], ALU.mult, ALU.mult,
                             r=[B_ysb, B_st3, B_gbc], w=[B_tmpy])
                      sc.tt("pool", x1t[sl][:], tmpy[:], xt3[sl][:], ALU.add, r=[B_tmpy, B_xt3[sl]], w=[B_x1t[sl]])
                      sc.dma("sp", out_d[r0 + tt * 128:r0 + (tt + 1) * 128, :], x1t[sl][:], out_sem[sl],
                             r=[B_x1t[sl]], w=[B_x1d[s][tt]])
                  sc.barrier()

              _ck("phase3")
              with ExitStack() as p4:
                  def sb4(name, shape, dt):
                      return sb(name, shape, dt, p4)

                  wdn = sb4("wdn", [128, 32, 1024], BF16)
                  gpl_bc = sb4("gpl_bc", [128, 1024], F32)
                  B_gbc4 = sc.buf("gbc4")
                  sc.dma("sp", gpl_bc[:], gpl_d[0:1, :].partition_broadcast(128), sc.dsem("gbc"), w=[B_gbc4])
                  B_wdn = sc.bufs_n("wdn", 4)
                  ds_wdn = sc.dsem("wdn")
                  wup = [oTa[:, 2 * i:2 * i + 2, :].rearrange("p a (k n) -> p (a k) n", n=512) for i in range(2)]
                  B_wup = sc.bufs_n("wup", 2)
                  ds_wup = [sc.dsem("wup0"), sc.dsem("wup1")]
                  def aTs(uc, lo=0, hi=512):
                      return hT[:, uc // 4, (uc % 4) * 512 + lo:(uc % 4) * 512 + hi]

                  B_aT = sc.bufs_n("aT", 32)
                  h2T = oTb[:, 0:2, :].rearrange("p a (k n) -> p (a k) n", n=512)
                  B_h2T = sc.buf("h2T")
                  x1g = [sb4("x1g%d" % i, [128, 1024], F32) for i in range(4)]
                  B_x1g = sc.bufs_n("x1g", 4)
                  xn4 = [oTb[:, 2, 0:1024], oTb[:, 2, 1024:2048]]
                  B_xn4 = sc.bufs_n("xn4", 2)
                  junk4 = sb4("junk4", [128, 1024], F32)
                  B_junk4 = sc.buf("junk4")
                  st4 = sb4("st4", [128, 4], F32)
                  B_st4 = sc.bufs_n("st4", 2)
                  rl = [sb4("rl0", [128, 512], F32), sb4("rl1", [128, 512], F32)]
                  B_rl = sc.bufs_n("rl", 2)
                  ysb4 = sb4("ysb4", [128, 1024], F32)
                  tmp4 = sb4("tmp4", [128, 1024], F32)
                  ot = [sb4("ot0", [128, 1024], F32), sb4("ot1", [128, 1024], F32)]
                  B_ysb4 = sc.buf("ysb4")
                  B_tmp4 = sc.buf("tmp4")
                  B_ot = sc.bufs_n("ot", 2)
                  ds_x1g = [sc.dsem("x1g%d" % i) for i in range(4)]

                  for kcg in range(4):
                      sc.dma("pool", wdn[:, kcg * 8:(kcg + 1) * 8, :], w_down_d[:, kcg * 8:(kcg + 1) * 8, :], ds_wdn,
                             w=[B_wdn[kcg]])
                  upcnt = 0
                  for g in range(4):
                      sc.dma("pool", wup[upcnt % 2], w_up_d[:, :, 0:512], ds_wup[upcnt % 2], w=[B_wup[upcnt % 2]])
                      for j in range(4):
                          tt = g * 4 + j
                          sc.dma("sp", x1g[j][:], out_d[r0 + tt * 128:r0 + (tt + 1) * 128, :], ds_x1g[j],
                                 r=[B_x1d[s][tt]], w=[B_x1g[j]])
                          sl = j % 2
                          norm_transpose(x1g[j][:], B_x1g[j], j * 128, xn4[sl], B_xn4[sl], st4[:, sl:sl + 1],
                                         st4[:, 2 + sl:3 + sl], B_st4[sl], junk4, B_junk4, sl, gpre2, h2T, B_h2T)
                      for ucg in range(8):
                          sl = upcnt % 2
                          upcnt += 1
                          if ucg + 1 < 8:
                              sc.dma("pool", wup[1 - sl], w_up_d[:, :, (ucg + 1) * 512:(ucg + 2) * 512],
                                     ds_wup[1 - sl], w=[B_wup[1 - sl]])
                          for ucl in range(4):
                              uc = ucg * 4 + ucl
                              b = 2 + (uc % 2)
                              for kc in range(8):
                                  sc.mm(ps[:, b, :], wup[sl][:, kc, ucl * 128:(ucl + 1) * 128], h2T[:, kc, :], kc == 0,
                                        kc == 7, r=[B_wup[sl], B_h2T], w=[B_ps[b]])
                              sc.act(rl[uc % 2][:], ps[:, b, :], AF.Relu, r=[B_ps[b]], w=[B_rl[uc % 2]])
                              sc.tt("pool", aTs(uc), rl[uc % 2][:], rl[uc % 2][:], ALU.mult, r=[B_rl[uc % 2]],
                                    w=[B_aT[uc]])
                      if g + 1 < 4:
                          pass
                      for j in range(4):
                          tt = g * 4 + j
                          yb = 4 + 2 * (j % 2)
                          for half in range(2):
                              for uc in range(32):
                                  sc.mm(ps[:, yb + half, :], aTs(uc, j * 128, (j + 1) * 128),
                                        wdn[:, uc, half * 512:(half + 1) * 512], uc == 0, uc == 31,
                                        r=[B_aT[uc], B_wdn[uc // 8]], w=[B_ps[yb + half]])
                          sc.act(ysb4[:, 0:512], ps[:, yb, :], AF.Copy, r=[B_ps[yb]], w=[B_ysb4])
                          sc.act(ysb4[:, 512:1024], ps[:, yb + 1, :], AF.Copy, r=[B_ps[yb + 1]], w=[B_ysb4])
                          sc.stt("dve", junk4[:], ysb4[:], 1.0, ysb4[:], ALU.mult, ALU.mult, r=[B_ysb4],
                                 w=[B_junk4, B_st4[0]], accum_out=st4[:, 0:1])
                          sc.act(st4[:, 2:3], st4[:, 0:1], AF.Sqrt, r=[B_st4[0], B_const], w=[B_st4[0]], bias=fcol[:, 4:5], scale=1.0 / D)
                          sc.op("dve", partial(nc.vector.reciprocal, st4[:, 2:3], st4[:, 2:3]), r=[B_st4[0]], w=[B_st4[0]])
                          sc.stt("dve", tmp4[:], ysb4[:], st4[:, 2:3], gpl_bc[:], ALU.mult, ALU.mult,
                                 r=[B_ysb4, B_st4[0], B_gbc4], w=[B_tmp4])
                          sc.tt("pool", ot[j % 2][:], tmp4[:], x1g[j][:], ALU.add, r=[B_tmp4, B_x1g[j]], w=[B_ot[j % 2]])
                          sc.dma("sp", out_d[r0 + tt * 128:r0 + (tt + 1) * 128, :], ot[j % 2][:], out_sem[j % 2],
                                 r=[B_ot[j % 2]], w=[B_x1d[s][tt]])
                  sc.barrier()

        except _Stop:
            raise
        n_ins, n_wait = sc.emit()
        build_nc.stats = (n_ins, n_wait)
    return nc


_NC_CACHE = {}


def kernel(x, positions, g_pre_mix, w_in, b_gate, g_q_norm, w_uq, g_kv_norm, w_ukv, w_branch_a, w_branch_b, w_out,
           g_post_mix, g_pre_mlp, w_up, w_down, g_post_mlp):
    x = np.asarray(x, dtype=np.float32)
    positions = np.asarray(positions, dtype=np.int32)
    args = [np.asarray(a, dtype=np.float32) for a in
            (w_in, w_uq, w_ukv, w_branch_a, w_branch_b, w_out, w_up, w_down, g_pre_mix, b_gate, g_q_norm,
             g_kv_norm, g_post_mix, g_pre_mlp, g_post_mlp)]
    shared = _host_weights(*args)
    shared.update(_host_consts())
    if "nc" not in _NC_CACHE:
        _NC_CACHE["nc"] = build_nc(NSEQ)
    nc = _NC_CACHE["nc"]
    in_maps = []
    for c in range(NCORES):
        m = dict(shared)
        m["x"] = np.ascontiguousarray(x[c * NSEQ:(c + 1) * NSEQ].reshape(NSEQ * S, D))
        m["pos"] = np.ascontiguousarray(positions[c * NSEQ:(c + 1) * NSEQ])
        in_maps.append(m)
    res = run_bass_kernel_spmd(nc, in_maps, core_ids=list(range(NCORES)))
    out = np.concatenate([np.asarray(r["out"]).reshape(NSEQ, S, D) for r in res.results], axis=0)
    return out.astype(np.float32, copy=False)
```

```python
import math
from contextlib import ExitStack
from functools import partial

import numpy as np
import concourse.bass as bass
import concourse.mybir as mybir
from concourse.bass_utils import run_bass_kernel_spmd

F32 = mybir.dt.float32
BF16 = mybir.dt.bfloat16
I32 = mybir.dt.int32
ALU = mybir.AluOpType
AF = mybir.ActivationFunctionType
AX = mybir.AxisListType

NCORES = 8
NSEQ = 4
S = 2048
D = 1024
EPS = 1e-6
NEG = -30000.0
PIPE = 3
THETA = 500000.0


class _Stop(Exception):
    pass


_STOP = [None, None, None]


def _ck(name):
    if _STOP[0] == name:
        sc = _STOP[1]
        sc.barrier()
        build_nc.stats = sc.emit()
        raise _Stop()


class Buf:
    __slots__ = ("name", "last_w", "readers")

    def __init__(self, name):
        self.name = name
        self.last_w = None
        self.readers = []


class DSem:
    __slots__ = ("h", "count", "last", "name")

    def __init__(self, h, name=""):
        self.name = name
        self.h = h
        self.count = 0
        self.last = None


class Ins:
    __slots__ = ("eng", "fn", "deps", "sig", "sigval", "dsem", "dval", "is_dma")

    def __init__(self, eng, fn):
        self.eng = eng
        self.fn = fn
        self.deps = []
        self.sig = False
        self.sigval = 0
        self.dsem = None
        self.dval = 0
        self.is_dma = False


class Sched:
    ENGS = ("pe", "act", "dve", "pool", "sp")

    def __init__(self, nc, stack):
        self.nc = nc
        self.stack = stack
        self.eobj = {"pe": nc.tensor, "act": nc.scalar, "dve": nc.vector, "pool": nc.gpsimd, "sp": nc.sync}
        self.esem = {e: stack.enter_context(nc.semaphore("es_" + e)) for e in self.ENGS}
        self.prog = []
        self.last = {e: None for e in self.ENGS}
        self.lastc = {e: None for e in self.ENGS}
        self.bufs = []
        self.dsems = []
        self._dsc = {}

    def buf(self, name):
        b = Buf(name)
        self.bufs.append(b)
        return b

    def bufs_n(self, name, n):
        return [self.buf("%s%d" % (name, i)) for i in range(n)]

    def dsem(self, name):
        name = name.split("%")[0]
        if name in self._dsc:
            return self._dsc[name]
        d = self._dsc[name] = DSem(self.stack.enter_context(self.nc.semaphore("ds_" + name)), name)
        self.dsems.append(d)
        return d

    @staticmethod
    def _need(src, dst, kind):
        if src.is_dma or dst.is_dma:
            return True
        if src.eng != dst.eng:
            return True
        if src.eng == "pe":
            return False
        return True

    def _track(self, ins, r, w):
        deps = ins.deps

        def add(d):
            if d.is_dma:
                deps.append((d.dsem, d.dsem.count))
            else:
                d.sig = True
                deps.append(d)

        for b in r:
            lw = b.last_w
            if lw is not None and self._need(lw, ins, "RAW"):
                add(lw)
        for b in w:
            lw = b.last_w
            if lw is not None and self._need(lw, ins, "WAW"):
                add(lw)
            for rd in b.readers:
                if rd is not ins and self._need(rd, ins, "WAR"):
                    add(rd)
        for b in w:
            b.last_w = ins
            b.readers = []
        for b in r:
            if b.last_w is ins:
                continue
            if not ins.is_dma:
                b.readers = [x for x in b.readers if x.is_dma or x.eng != ins.eng]
            b.readers.append(ins)
        self.prog.append(ins)
        self.last[ins.eng] = ins
        if not ins.is_dma:
            self.lastc[ins.eng] = ins

    def op(self, eng, fn, r=(), w=()):
        ins = Ins(eng, fn)
        self._track(ins, r, w)
        return ins

    def dma(self, eng, out, in_, ds, r=(), w=()):
        ins = Ins(eng, partial(self.eobj[eng].dma_start, out=out, in_=in_))
        if not ds.name.endswith("@" + eng):
            ds = self.dsem(ds.name.split("@")[0] + "@" + eng)
        ins.is_dma = True
        ins.dsem = ds
        self._track(ins, r, w)
        ds.count += 16
        ins.dval = ds.count
        ds.last = ins
        return ins

    def barrier(self):
        lasts = [self.lastc[e] for e in self.ENGS if self.lastc[e] is not None]
        dl = [d.last for d in self.dsems if d.last is not None]
        for e in self.ENGS:
            ins = Ins(e, None)
            for l in lasts:
                if l.eng != e:
                    ins.deps.append(l)
                    l.sig = True
            ins.deps.extend((d.dsem, d.dsem.count) for d in dl)
            self.prog.append(ins)
        for b in self.bufs:
            b.last_w = None
            b.readers = []

    def emit(self):
        cnt = {e: 0 for e in self.ENGS}
        for ins in self.prog:
            if ins.sig and not ins.is_dma and ins.fn is not None:
                cnt[ins.eng] += 1
                ins.sigval = cnt[ins.eng]
        waited = {}
        nwait = 0
        for ins in self.prog:
            E = self.eobj[ins.eng]
            for d in ins.deps:
                if isinstance(d, tuple):
                    sem, val = d[0].h, d[1]
                else:
                    sem, val = self.esem[d.eng], d.sigval
                key = (ins.eng, id(sem))
                if waited.get(key, 0) >= val:
                    continue
                waited[key] = val
                E.wait_ge(sem, val)
                nwait += 1
            if ins.fn is None:
                continue
            bi = ins.fn()
            if ins.is_dma:
                bi.then_inc(ins.dsem.h, 16)
            elif ins.sig:
                bi.then_inc(self.esem[ins.eng], 1)
        return len(self.prog), nwait

    def mm(self, out, lhsT, rhs, start, stop, r, w):
        return self.op("pe", partial(self.nc.tensor.matmul, out, lhsT, rhs, start=start, stop=stop), r, w)

    def tr(self, out, in_, ident, r, w):
        return self.op("pe", partial(self.nc.tensor.transpose, out, in_, ident), r, w)

    def act(self, out, in_, func, r, w, **kw):
        return self.op("act", partial(self.nc.scalar.activation, out, in_, func, **kw), r, w)

    def tt(self, eng, out, in0, in1, op, r, w):
        return self.op(eng, partial(self.eobj[eng].tensor_tensor, out=out, in0=in0, in1=in1, op=op), r, w)

    def ts(self, eng, out, in0, s1, s2, op0, op1, r, w):
        if op1 is None:
            fn = partial(self.eobj[eng].tensor_scalar, out, in0, s1, None, op0)
        else:
            fn = partial(self.eobj[eng].tensor_scalar, out, in0, s1, s2, op0, op1)
        return self.op(eng, fn, r, w)

    def stt(self, eng, out, in0, scalar, in1, op0, op1, r, w, accum_out=None):
        if accum_out is None:
            fn = partial(self.eobj[eng].scalar_tensor_tensor, out, in0, scalar, in1, op0, op1)
        else:
            fn = partial(self.eobj[eng].scalar_tensor_tensor, out, in0, scalar, in1, op0, op1, accum_out)
        return self.op(eng, fn, r, w)

    def cp(self, eng, out, in_, r, w):
        return self.op(eng, partial(self.eobj[eng].tensor_copy, out, in_), r, w)

    def memset(self, eng, ap, val, r, w):
        return self.op(eng, partial(self.eobj[eng].memset, ap, val), r, w)


def _host_consts():
    c = {}
    c["ident"] = np.eye(128, dtype=np.float32)
    p = np.arange(128)[:, None]
    q = np.arange(128)[None, :]
    c["tri"] = np.where(p <= q, 0.0, NEG).astype(np.float32)
    k = np.arange(S)
    n = np.arange(8)
    c["kind"] = (k[None, :] // 256 == n[:, None]).astype(np.float32)
    c["qbias0"] = np.where(n[:, None] <= (k[None, :1024] // 256), 0.0, NEG).astype(np.float32)
    own = (np.arange(8, 16) // 2)
    mn = np.zeros((8, 2, 8), np.float32)
    mm_ = np.zeros((8, 2, 8), np.float32)
    for i in range(8):
        for nn in range(8):
            if nn == own[i]:
                mn[i, :, nn] = 1e30
            elif nn > own[i]:
                mn[i, :, nn] = -1e30
            if nn >= own[i]:
                mm_[i, :, nn] = -1e30
    c["mask_n"] = np.broadcast_to(mn.reshape(1, 128), (128, 128)).copy()
    c["mask_m"] = np.broadcast_to(mm_.reshape(1, 128), (128, 128)).copy()
    fcol = np.zeros((128, 8), np.float32)
    invm = THETA ** (-np.arange(8, dtype=np.float32) * (2.0 / 16))
    invl = THETA ** (-np.arange(16, dtype=np.float32) * (2.0 / 32))
    fcol[:, 0] = 1.0 / (2 * math.pi)
    fcol[:, 1] = 0.0
    for j in range(16):
        fcol[j, 0] = invm[j % 8] / (2 * math.pi)
        fcol[j, 1] = 0.5 if j < 8 else 0.0
    for j in range(32):
        fcol[32 + j, 0] = invl[j % 16] / (2 * math.pi)
        fcol[32 + j, 1] = 0.5 if j < 16 else 0.0
    fcol[:, 2] = 0.25
    fcol[:, 4] = EPS
    c["fcol"] = fcol
    return c


def _host_weights(w_in, w_uq, w_ukv, w_branch_a, w_branch_b, w_out, w_up, w_down, g_pre_mix, b_gate,
                  g_q_norm, g_kv_norm, g_post_mix, g_pre_mlp, g_post_mlp):
    w = {}
    wi = w_in[0]
    cols = []
    for pr in range(4):
        for base0 in (0, 512):
            for h in (2 * pr, 2 * pr + 1):
                b = base0 + h * 64
                cols.extend(range(b, b + 64))
                cols.extend(range(b + 8, b + 16))
                cols.extend(range(b, b + 8))
    w["w_qk"] = np.ascontiguousarray(wi[:, cols])
    w["w_v"] = np.ascontiguousarray(wi[:, 1024:1536])
    w["w_cq"] = np.ascontiguousarray(wi[:, 1536:1920])
    w["w_ckv"] = np.ascontiguousarray(wi[:, 1920:2176])
    kr = list(range(2176, 2208)) + list(range(2176 + 16, 2208)) + list(range(2176, 2176 + 16))
    w["w_kr"] = np.ascontiguousarray(wi[:, kr])
    w["w_gate"] = np.ascontiguousarray(wi[:, 2208:4256])
    uq = w_uq[0]
    cols = []
    for h in range(8):
        b = h * 96
        cols.extend(range(b + 64, b + 96))
        cols.extend(range(b + 80, b + 96))
        cols.extend(range(b + 64, b + 80))
        cols.extend(range(b, b + 64))
    w["w_uq"] = np.ascontiguousarray(uq[:, cols])
    ukv = w_ukv[0]
    wk = np.zeros((256, 8, 128), np.float32)
    wv = np.zeros((256, 8, 64), np.float32)
    for h in range(8):
        wk[:, h, 64:128] = ukv[:, h * 128:h * 128 + 64]
        wv[:, h, :] = ukv[:, h * 128 + 64:h * 128 + 128]
    w["w_ukvk"] = wk.reshape(256, 1024)
    w["w_ukvv"] = wv.reshape(256, 512)
    w["w_a"] = np.ascontiguousarray(w_branch_a[0])
    w["w_b"] = np.ascontiguousarray(w_branch_b[0])
    w["w_out"] = np.ascontiguousarray(w_out[0])
    w["w_up"] = np.ascontiguousarray(w_up[0])
    w["w_down"] = np.ascontiguousarray(w_down[0])

    def colsT(v, nk):
        return np.ascontiguousarray(v.reshape(nk, 128).T)

    w["gpre_T"] = colsT(g_pre_mix[0], 8)
    w["gpre2_T"] = colsT(g_pre_mlp[0], 8)
    w["gq_T"] = colsT(g_q_norm[0], 3)
    w["gkv_T"] = colsT(g_kv_norm[0], 2)
    w["bgate_T"] = colsT(b_gate[0], 16)
    w["gpm"] = np.ascontiguousarray(g_post_mix[0].reshape(1, 1024))
    w["gpl"] = np.ascontiguousarray(g_post_mlp[0].reshape(1, 1024))
    return {k: np.asarray(v, dtype=np.float32) for k, v in w.items()}


def build_nc(nseq=NSEQ, dbg=None):
    try:
        return _build_nc(nseq, dbg)
    except _Stop:
        return _STOP[2]


def _build_nc(nseq=NSEQ, dbg=None):
    nc = bass.Bass("TRN2", target_bir_lowering=False)
    _STOP[2] = nc
    NTOK = nseq * S

    def din(name, shape, dt=F32):
        return nc.dram_tensor(name, list(shape), dt, kind="ExternalInput").ap()

    x_d = din("x", [NTOK, D])
    pos_d = din("pos", [nseq, S], I32)
    w_qk_d = din("w_qk", [1024, 1280]).rearrange("(kc p) n -> p kc n", p=128)
    w_v_d = din("w_v", [1024, 512]).rearrange("(kc p) n -> p kc n", p=128)
    w_cq_d = din("w_cq", [1024, 384]).rearrange("(kc p) n -> p kc n", p=128)
    w_ckv_d = din("w_ckv", [1024, 256]).rearrange("(kc p) n -> p kc n", p=128)
    w_kr_d = din("w_kr", [1024, 64]).rearrange("(kc p) n -> p kc n", p=128)
    w_gate_d = din("w_gate", [1024, 2048]).rearrange("(kc p) n -> p kc n", p=128)
    w_uq_d = din("w_uq", [384, 1024]).rearrange("(kc p) n -> p kc n", p=128)
    w_ukvk_d = din("w_ukvk", [256, 1024]).rearrange("(kc p) n -> p kc n", p=128)
    w_ukvv_d = din("w_ukvv", [256, 512]).rearrange("(kc p) n -> p kc n", p=128)
    w_a_d = din("w_a", [512, 1024]).rearrange("(kc p) n -> p kc n", p=128)
    w_b_d = din("w_b", [512, 1024]).rearrange("(kc p) n -> p kc n", p=128)
    w_out_d = din("w_out", [1024, 1024]).rearrange("(kc p) n -> p kc n", p=128)
    w_up_d = din("w_up", [1024, 4096]).rearrange("(kc p) n -> p kc n", p=128)
    w_down_d = din("w_down", [4096, 1024]).rearrange("(kc p) n -> p kc n", p=128)
    gpre_d = din("gpre_T", [128, 8])
    gpre2_d = din("gpre2_T", [128, 8])
    gq_d = din("gq_T", [128, 3])
    gkv_d = din("gkv_T", [128, 2])
    bgate_d = din("bgate_T", [128, 16])
    gpm_d = din("gpm", [1, 1024])
    gpl_d = din("gpl", [1, 1024])
    ident_d = din("ident", [128, 128])
    tri_d = din("tri", [128, 128])
    kind_d = din("kind", [8, S])
    qbias0_d = din("qbias0", [8, 1024])
    maskn_d = din("mask_n", [128, 128])
    maskm_d = din("mask_m", [128, 128])
    fcol_d = din("fcol", [128, 8])
    out_d = nc.dram_tensor("out", [NTOK, D], F32, kind="ExternalOutput").ap()
    dbg_d = {}
    if dbg:
        for name, shape in dbg.items():
            dbg_d[name] = nc.dram_tensor("dbg_" + name, list(shape), BF16, kind="ExternalOutput").ap()

    with ExitStack() as st:
        sc = Sched(nc, st)
        _STOP[1] = sc

        uniq = [0]

        def sb(name, shape, dt, stack=st):
            uniq[0] += 1
            return stack.enter_context(nc.sbuf_tensor("s%d_%s" % (uniq[0], name), list(shape), dt))

        ps = st.enter_context(nc.psum_tensor("psum_all", [128, 8, 512], F32))
        psb = ps.bitcast(BF16) if hasattr(ps, "bitcast") else None
        B_ps = sc.bufs_n("ps", 8)

        ident = sb("ident", [128, 128], BF16)
        tri = sb("tri", [128, 128], BF16)
        gpre = sb("gpre", [128, 8], F32)
        gpre2 = sb("gpre2", [128, 8], F32)
        gq = sb("gq", [128, 3], F32)
        gkv = sb("gkv", [128, 2], F32)
        bgate = sb("bgate", [128, 16], F32)
        fcol = sb("fcol", [128, 8], F32)
        ones_bf = sb("ones_bf", [128, 128], BF16)
        hT = sb("hT", [128, 8, S], BF16)
        oTa = sb("oTa", [128, 4, S], BF16)
        oTb = sb("oTb", [128, 4, S], BF16)
        B_const = sc.buf("const")
        B_hT = sc.bufs_n("hT", 4)
        B_oTa = [sc.bufs_n("oTa%d_" % p, 4) for p in range(4)]
        B_oTb = [sc.bufs_n("oTb%d_" % p, 4) for p in range(4)]
        ds_const = sc.dsem("const")

        for (t, d) in ((gpre, gpre_d), (gpre2, gpre2_d), (gq, gq_d), (gkv, gkv_d), (bgate, bgate_d),
                       (fcol, fcol_d)):
            sc.dma("sp", t[:], d[:, :], ds_const, w=[B_const])
        sc.dma("pool", ident[:], ident_d[:, :], ds_const, w=[B_const])
        sc.dma("pool", tri[:], tri_d[:, :], ds_const, w=[B_const])
        sc.memset("dve", ones_bf[:], 1.0, r=[], w=[B_const])
        sc.barrier()

        xt_sem = [sc.dsem("xt0"), sc.dsem("xt1")]
        out_sem = [sc.dsem("o0"), sc.dsem("o1")]
        B_x1d = [sc.bufs_n("x1d%d_" % s, 16) for s in range(nseq)]

        def norm_transpose(src_tile, B_src, tt_col, xn, B_xn, ssq, rstd, B_st, junk, B_junk, bank, gcol, dstT,
                           B_dst):
            sc.stt("dve", junk[:], src_tile, 1.0, src_tile, ALU.mult, ALU.mult, r=[B_src], w=[B_junk, B_st],
                   accum_out=ssq[:, 0:1])
            sc.act(rstd[:, 0:1], ssq[:, 0:1], AF.Sqrt, r=[B_st, B_const], w=[B_st], bias=fcol[:, 4:5], scale=1.0 / D)
            sc.op("dve", partial(nc.vector.reciprocal, rstd[:, 0:1], rstd[:, 0:1]), r=[B_st], w=[B_st])
            sc.ts("dve", xn[:], src_tile, rstd[:, 0:1], None, ALU.mult, None, r=[B_src, B_st], w=[B_xn])
            pT = psb[:, bank, :]
            for kc in range(8):
                sc.tr(pT[:, kc * 128:(kc + 1) * 128], xn[:, kc * 128:(kc + 1) * 128], ident[:], r=[B_xn, B_const],
                      w=[B_ps[bank]])
            sc.tt("dve", dstT[:, :, tt_col:tt_col + 128], pT.rearrange("p (k t) -> p k t", k=8),
                  gcol[:, :].unsqueeze(2).broadcast_to([128, 8, 128]), ALU.mult, r=[B_ps[bank], B_const],
                  w=[B_dst])

        try:
          for s in range(nseq):
              r0 = s * S
              with ExitStack() as p2:
                  def sb2(name, shape, dt):
                      return sb(name, shape, dt, p2)

                  COS = sb2("COS", [128, S], F32)
                  SIN = sb2("SIN", [128, S], F32)
                  p1 = p2.enter_context(ExitStack())

                  def sb1(name, shape, dt):
                      return sb(name, shape, dt, p1)

                  xt = [sb1("xt0", [128, 1024], F32), sb1("xt1", [128, 1024], F32)]
                  xn = [sb1("xn0", [128, 1024], BF16), sb1("xn1", [128, 1024], BF16)]
                  junk = sb1("junk", [128, 1024], F32)
                  ssq = sb1("ssq", [128, 2], F32)
                  rstd = sb1("rstd", [128, 2], F32)
                  B_xt = sc.bufs_n("xt", 2)
                  B_xn = sc.bufs_n("xn", 2)
                  B_junk = sc.buf("junk")
                  B_st = sc.bufs_n("st", 2)
                  posi = sb1("posi", [128, S], I32)
                  B_tab = sc.buf("tab")
                  B_posi = sc.buf("posi")
                  ds_pos = sc.dsem("pos")

                  sc.dma("sp", posi[:], pos_d[s:s + 1, :].partition_broadcast(128), ds_pos, w=[B_posi])
                  kint = sb1("kint", [128, 1024], I32)
                  kflt = sb1("kflt", [128, 1024], F32)
                  B_kint = sc.buf("kint")
                  for hh in range(2):
                      cs = slice(hh * 1024, (hh + 1) * 1024)
                      sc.cp("dve", junk[:, :], posi[:, cs], r=[B_posi], w=[B_junk])
                      for (tab, phc) in ((SIN, 1), (COS, 2)):
                          sc.ts("dve", tab[:, cs], junk[:, :], fcol[:, 0:1], fcol[:, phc:phc + 1], ALU.mult, ALU.add,
                                r=[B_junk, B_const], w=[B_tab])
                          sc.cp("dve", kint[:, :], tab[:, cs], r=[B_tab], w=[B_kint])
                          sc.cp("dve", kflt[:, :], kint[:, :], r=[B_kint], w=[B_kint])
                          sc.tt("dve", tab[:, cs], tab[:, cs], kflt[:, :], ALU.subtract, r=[B_tab, B_kint], w=[B_tab])
                          sc.act(tab[:, cs], tab[:, cs], AF.Sin, r=[B_tab], w=[B_tab], scale=6.2831845)

                  _ck("tables")
                  for tt in range(16):
                      sl = tt % 2
                      sc.dma("sp", xt[sl][:], x_d[r0 + tt * 128:r0 + (tt + 1) * 128, :], xt_sem[sl], w=[B_xt[sl]])
                      norm_transpose(xt[sl][:], B_xt[sl], tt * 128, xn[sl], B_xn[sl], ssq[:, sl:sl + 1],
                                     rstd[:, sl:sl + 1], B_st[sl], junk, B_junk, sl, gpre, hT, B_hT[tt // 4])

                  sc.barrier()
                  p1.close()

                  _ck("phase1")
                  QT = [sb2("QT0", [128, S], BF16), sb2("QT1", [128, S], BF16)]
                  KT = [sb2("KT0", [128, S], BF16), sb2("KT1", [128, S], BF16)]
                  B_QT = [sc.bufs_n("QT%d_" % i, 4) for i in range(2)]
                  B_KT = [sc.bufs_n("KT%d_" % i, 4) for i in range(2)]
                  B_QTx = sc.bufs_n("QTx", 2)
                  B_KTx = sc.bufs_n("KTx", 2)
                  Vaug = sb2("Vaug", [128, 16, 4, 192], BF16)
                  B_V = sc.bufs_n("V", 16)
                  B_Vones = sc.buf("Vones")
                  PT = [sb2("PT%d" % i, [128, 512], BF16) for i in range(4)]
                  B_PT = sc.bufs_n("PT", 4)
                  rtmp = [sb2("rtmp%d" % i, [128, 512], F32) for i in range(2)]
                  B_rtmp = sc.bufs_n("rtmp", 2)
                  t1 = sb2("t1", [128, 512], F32)
                  t2 = sb2("t2", [128, 512], F32)
                  B_t1 = sc.buf("t1")
                  B_t2 = sc.buf("t2")
                  B_wv = sc.buf("wv")
                  ds_wv = sc.dsem("wv")
                  pm = p2.enter_context(ExitStack())

                  def sb2m(name, shape, dt):
                      return sb(name, shape, dt, pm)

                  wv = sb2m("wv", [128, 8, 512], BF16)

                  wqk = [sb2m("wqk0", [128, 8, 320], BF16), sb2m("wqk1", [128, 8, 320], BF16)]
                  B_wqk = sc.bufs_n("wqk", 2)
                  ds_wqk = [sc.dsem("wqk0"), sc.dsem("wqk1")]
                  kmT = sb2m("kmT", [128, 2, 8], F32)
                  kmTb = sb2m("kmTb", [128, 2, 8], BF16)
                  B_km = sc.bufs_n("km", 2)
                  gn = sb2m("gn", [128, 128], F32)
                  gm = sb2m("gm", [128, 128], F32)
                  rk = sb2m("rk", [128, 128], F32)
                  cmpt = sb2m("cmpt", [128, 128], F32)
                  maskn = sb2m("maskn", [128, 128], F32)
                  maskm = sb2m("maskm", [128, 128], F32)
                  bpad = sb2m("bpad", [128, 8, 2, 72], BF16)
                  B_g = sc.buf("gating")
                  B_bpad = sc.buf("bpad")
                  ds_misc = sc.dsem("misc")
                  sc.dma("sp", maskn[:], maskn_d[:, :], ds_misc, w=[B_g])
                  sc.dma("sp", maskm[:], maskm_d[:, :], ds_misc, w=[B_g])
                  sc.memset("pool", bpad[:], 0.0, r=[], w=[B_bpad])
                  sc.memset("pool", Vaug[:, :, :, 64:128], 1.0, r=[], w=[B_Vones])

                  state = {"sb": 0, "pt": 0, "acc": 0, "pendq": [], "rt": 0}

                  def flush_pv(keep=0):
                      while len(state["pendq"]) > keep:
                          _flush_one(state["pendq"].pop(0))

                  def _flush_one(pd):
                      (hl, pr, kt, c0, ptb, acc, first, last, qi, oT, B_oT) = pd
                      sc.mm(ps[:, acc, c0:512], Vaug[:, kt, pr, hl * 64:hl * 64 + 128], PT[ptb][:, c0:512],
                            first, last, r=[B_V[kt], B_Vones, B_PT[ptb]], w=[B_ps[acc]])
                      if last:
                          ri = state["rt"]
                          state["rt"] = 1 - ri
                          o_lo, d_lo = (0, 64) if hl == 0 else (64, 0)
                          sc.op("dve", partial(nc.vector.reciprocal, rtmp[ri][o_lo:o_lo + 64, :],
                                               ps[d_lo:d_lo + 64, acc, :]), r=[B_ps[acc]], w=[B_rtmp[ri]])
                          sc.tt("dve", oT[o_lo:o_lo + 64, pr, qi * 512:(qi + 1) * 512], ps[o_lo:o_lo + 64, acc, :],
                                rtmp[ri][o_lo:o_lo + 64, :], ALU.mult, r=[B_ps[acc], B_rtmp[ri]], w=[B_oT[pr][qi]])

                  def attention(hl, pr, R, scale, oT, B_oT):
                      for qi in range(4):
                          q0 = qi * 512
                          nkt = 4 * qi + 4
                          acc = 5 + state["acc"]
                          state["acc"] = 1 - state["acc"]
                          for kt in range(nkt):
                              rdiag = kt - 4 * qi
                              c0 = 128 * rdiag if rdiag > 0 else 0
                              sbk = (2, 3, 4, 0)[state["sb"]]
                              state["sb"] = (state["sb"] + 1) % 4
                              ptb = state["pt"]
                              state["pt"] = (state["pt"] + 1) % 4
                              sc.mm(ps[:, sbk, c0:512], KT[hl][0:R, kt * 128:(kt + 1) * 128],
                                    QT[hl][0:R, q0 + c0:q0 + 512], True, rdiag < 0,
                                    r=[B_KT[hl][kt // 4], B_KTx[hl], B_QT[hl][qi], B_QTx[hl]], w=[B_ps[sbk]])
                              if rdiag >= 0:
                                  sc.mm(ps[:, sbk, c0:c0 + 128], ident[:], tri[:], False, True, r=[B_const],
                                        w=[B_ps[sbk]])
                              sc.act(PT[ptb][:, c0:512], ps[:, sbk, c0:512], AF.Exp, r=[B_ps[sbk]], w=[B_PT[ptb]],
                                     scale=scale)
                              flush_pv(keep=PIPE - 1)
                              state["pendq"].append((hl, pr, kt, c0, ptb, acc, kt == 0, kt == nkt - 1, qi, oT, B_oT))

                  def rope_rows(dst, nrow, pst, swap_lo, cos_lo, g, B_dst):
                      cs = slice(g * 512, (g + 1) * 512)
                      sc.tt("dve", t1[0:nrow, :], pst[swap_lo:swap_lo + nrow, :], SIN[cos_lo:cos_lo + nrow, cs],
                            ALU.mult, r=[B_ps_cur[0], B_tab, B_dst], w=[B_t1])
                      sc.tt("dve", t2[0:nrow, :], pst[0:nrow, :], COS[cos_lo:cos_lo + nrow, cs], ALU.mult,
                            r=[B_ps_cur[0], B_tab, B_dst], w=[B_t2])
                      sc.tt("dve", dst[0:nrow, cs], t1[0:nrow, :], t2[0:nrow, :], ALU.add, r=[B_t1, B_t2],
                            w=[B_dst])

                  B_ps_cur = [None]
                  pbank = {"i": 0}

                  def next_pbank():
                      b = pbank["i"]
                      pbank["i"] = 1 - b
                      B_ps_cur[0] = B_ps[b]
                      return b

                  sc.dma("pool", wv[:], w_v_d[:, :, :], ds_wv, w=[B_wv])
                  sc.dma("pool", wqk[0][:], w_qk_d[:, :, 0:320], ds_wqk[0], w=[B_wqk[0]])
                  _ck("p2init")
                  for tt in range(16):
                      b = next_pbank()
                      for kc in range(8):
                          sc.mm(ps[:, b, :], hT[:, kc, tt * 128:(tt + 1) * 128], wv[:, kc, :], kc == 0, kc == 7,
                                r=[B_hT[tt // 4], B_wv], w=[B_ps[b]])
                      src = ps[:, b, :].rearrange("p (a e d) -> p a e d", a=4, e=2)
                      sc.act(Vaug[:, tt, :, 0:64], src[:, :, 0, :], AF.Copy, r=[B_ps[b]], w=[B_V[tt]])
                      sc.cp("dve", Vaug[:, tt, :, 128:192], src[:, :, 1, :], r=[B_ps[b]], w=[B_V[tt]])
                  for pr in range(4):
                      wsl = pr % 2
                      if pr + 1 < 4:
                          sc.dma("pool", wqk[1 - wsl][:], w_qk_d[:, :, (pr + 1) * 320:(pr + 2) * 320], ds_wqk[1 - wsl],
                                 w=[B_wqk[1 - wsl]])
                      for hl in (range(2) if pr == 0 else ()):
                          sc.dma("pool", KT[hl][64:72, :], kind_d[:, :], ds_misc, w=[B_KTx[hl]])
                          sc.dma("pool", QT[hl][64:72, 0:1024], qbias0_d[:, :], ds_misc, w=[B_QTx[hl]])
                      _ck("mobaV")
                      for j in range(4):
                          hl = j % 2
                          dst = QT[hl] if j < 2 else KT[hl]
                          B_dst = B_QT[hl] if j < 2 else B_KT[hl]
                          for g in range(4):
                              b = next_pbank()
                              for kc in range(8):
                                  sc.mm(ps[0:80, b, :], wqk[wsl][:, kc, j * 80:(j + 1) * 80],
                                        hT[:, kc, g * 512:(g + 1) * 512], kc == 0, kc == 7,
                                        r=[B_wqk[wsl], B_hT[g]], w=[B_ps[b]])
                              sc.act(dst[0:64, g * 512:(g + 1) * 512], ps[0:64, b, :], AF.Copy, r=[B_ps[b]],
                                     w=[B_dst[g]])
                              rope_rows(dst, 16, ps[:, b, :], 64, 0, g, B_dst[g])
                      _ck("mobaproj")
                      for hl in range(2):
                          sc.op("dve", partial(nc.vector.tensor_reduce, kmT[0:64, hl, :],
                                               KT[hl][0:64, :].rearrange("p (n k) -> p n k", n=8), AX.X, ALU.add),
                                r=B_KT[hl], w=[B_km[hl]])
                          sc.ts("dve", kmTb[0:64, hl, :], kmT[0:64, hl, :], 1.0 / 256, None, ALU.mult, None,
                                r=[B_km[hl]], w=[B_km[hl]])
                      gb = 7
                      for qt in range(8):
                          for hl in range(2):
                              sc.mm(ps[:, gb, qt * 16 + hl * 8:qt * 16 + hl * 8 + 8],
                                    QT[hl][0:64, (8 + qt) * 128:(9 + qt) * 128], kmTb[0:64, hl, :], True, True,
                                    r=[B_QT[hl][2 + qt // 4], B_km[hl]], w=[B_ps[gb]])
                      sc.tt("dve", gn[:], ps[:, gb, 0:128], maskn[:], ALU.add, r=[B_ps[gb], B_g], w=[B_g])
                      sc.tt("dve", gm[:], ps[:, gb, 0:128], maskm[:], ALU.add, r=[B_ps[gb], B_g], w=[B_g])
                      gn3 = gn[:].rearrange("p (a n) -> p a n", n=8)
                      gm3 = gm[:].rearrange("p (a n) -> p a n", n=8)
                      rk3 = rk[:].rearrange("p (a n) -> p a n", n=8)
                      cm3 = cmpt[:].rearrange("p (a n) -> p a n", n=8)
                      for m in range(7):
                          dst3 = rk3 if m == 0 else cm3
                          sc.tt("dve", dst3, gm3[:, :, m:m + 1].broadcast_to([128, 16, 8]), gn3, ALU.is_gt, r=[B_g],
                                w=[B_g])
                          if m > 0:
                              sc.tt("dve", rk[:], rk[:], cmpt[:], ALU.add, r=[B_g], w=[B_g])
                      sc.ts("dve", bpad[:, :, :, 64:72], rk[:].rearrange("p (q h n) -> p q h n", q=8, h=2), 3.0, NEG,
                            ALU.is_ge, ALU.mult, r=[B_g], w=[B_bpad])
                      for hl in range(2):
                          for gg in range(2):
                              for q4 in range(4):
                                  qt = gg * 4 + q4
                                  sc.mm(ps[0:72, gb, q4 * 128:(q4 + 1) * 128], bpad[:, qt, hl, :], ident[:], True, True,
                                        r=[B_bpad, B_const], w=[B_ps[gb]])
                              sc.cp("dve", QT[hl][64:72, 1024 + gg * 512:1024 + (gg + 1) * 512], ps[64:72, gb, :],
                                    r=[B_ps[gb]], w=[B_QTx[hl]])
                      _ck("mobagate")
                      for hl in range(2):
                          attention(hl, pr, 72, 0.125, oTa, B_oTa)
                      flush_pv()

                  sc.barrier()
                  pm.close()
                  _ck("moba")
                  cqT = sb2("cqT", [128, 3, S], BF16)
                  ckvT = sb2("ckvT", [128, 2, S], BF16)
                  kropeT = sb2("kropeT", [32, S], BF16)
                  sqt = [sb2("sqt0", [128, 512], BF16), sb2("sqt1", [128, 512], BF16)]
                  rbc = sb2("rbc", [128, 512], F32)
                  B_cq = sc.bufs_n("cq", 4)
                  B_ckv = sc.bufs_n("ckv", 4)
                  B_krope = sc.bufs_n("krope", 4)
                  B_sqt = sc.bufs_n("sqt", 2)
                  B_rbc = sc.buf("rbc")
                  wc = sb2("wc", [128, 8, 384], BF16)
                  B_wc = sc.buf("wc")
                  ds_wc = sc.dsem("wc")
                  wuq = sb2("wuq", [128, 3, 256], BF16)
                  wukk = sb2("wukk", [128, 2, 256], BF16)
                  wvb = sb2("wvb", [128, 2, 512], BF16)
                  B_wu = sc.buf("wu")
                  ds_wu = sc.dsem("wu")
                  sc.dma("pool", wc[:, :, 0:384], w_cq_d[:, :, :], ds_wc, w=[B_wc])
                  sc.dma("pool", wvb[:], w_ukvv_d[:, :, :], ds_wv, w=[B_wv])

                  def latent(nch, col0, gcol, dstT, B_dst, g, nfeat):
                      cs = slice(g * 512, (g + 1) * 512)
                      sbank = 7
                      for c in range(nch):
                          b = c
                          for kc in range(8):
                              sc.mm(ps[:, b, :], wc[:, kc, col0 + c * 128:col0 + (c + 1) * 128], hT[:, kc, cs],
                                    kc == 0, kc == 7, r=[B_wc, B_hT[g]], w=[B_ps[b]])
                          sq = sqt[c % 2]
                          sc.act(sq[:], ps[:, b, :], AF.Square, r=[B_ps[b]], w=[B_sqt[c % 2]])
                          sc.mm(ps[:, sbank, :], ones_bf[:], sq[:], c == 0, c == nch - 1, r=[B_const, B_sqt[c % 2]],
                                w=[B_ps[sbank]])
                      sc.act(rbc[:], ps[:, sbank, :], AF.Sqrt, r=[B_ps[sbank], B_const], w=[B_rbc], bias=fcol[:, 4:5],
                             scale=1.0 / nfeat)
                      sc.op("dve", partial(nc.vector.reciprocal, rbc[:], rbc[:]), r=[B_rbc], w=[B_rbc])
                      for c in range(nch):
                          sc.stt("dve", dstT[:, c, cs], ps[:, c, :], gcol[:, c:c + 1], rbc[:], ALU.mult, ALU.mult,
                                 r=[B_ps[c], B_rbc, B_const], w=[B_dst[g]])

                  for g in range(4):
                      latent(3, 0, gq, cqT, B_cq, g, 384.0)
                  sc.dma("pool", wc[:, :, 0:256], w_ckv_d[:, :, :], ds_wc, w=[B_wc])
                  for g in range(4):
                      latent(2, 0, gkv, ckvT, B_ckv, g, 256.0)
                  sc.dma("pool", wc[:, :, 0:64], w_kr_d[:, :, :], ds_wc, w=[B_wc])
                  for g in range(4):
                      b = next_pbank()
                      for kc in range(8):
                          sc.mm(ps[0:64, b, :], wc[:, kc, 0:64], hT[:, kc, g * 512:(g + 1) * 512], kc == 0, kc == 7,
                                r=[B_wc, B_hT[g]], w=[B_ps[b]])
                      rope_rows(kropeT, 32, ps[:, b, :], 32, 32, g, B_krope[g])
                  _ck("mlaprep")
                  for tt in range(16):
                      b = next_pbank()
                      for kc in range(2):
                          sc.mm(ps[:, b, :], ckvT[:, kc, tt * 128:(tt + 1) * 128], wvb[:, kc, :], kc == 0, kc == 1,
                                r=[B_ckv[tt // 4], B_wv], w=[B_ps[b]])
                      src = ps[:, b, :].rearrange("p (a e d) -> p a e d", a=4, e=2)
                      sc.act(Vaug[:, tt, :, 0:64], src[:, :, 0, :], AF.Copy, r=[B_ps[b]], w=[B_V[tt]])
                      sc.cp("dve", Vaug[:, tt, :, 128:192], src[:, :, 1, :], r=[B_ps[b]], w=[B_V[tt]])
                  _ck("mlaV")
                  for pr in range(4):
                      sc.dma("pool", wuq[:], w_uq_d[:, :, pr * 256:(pr + 1) * 256], ds_wu, w=[B_wu])
                      sc.dma("pool", wukk[:], w_ukvk_d[:, :, pr * 256:(pr + 1) * 256], ds_wu, w=[B_wu])
                      _ck("mp1")
                      for hl in range(2):
                          h = 2 * pr + hl
                          for g in range(4):
                              cs = slice(g * 512, (g + 1) * 512)
                              b = next_pbank()
                              for kc in range(3):
                                  sc.mm(ps[:, b, :], wuq[:, kc, hl * 128:(hl + 1) * 128], cqT[:, kc, cs], kc == 0, kc == 2,
                                        r=[B_wu, B_cq[g]], w=[B_ps[b]])
                              _ck("mp2a")
                              sc.act(QT[hl][:, cs], ps[:, b, :], AF.Copy, r=[B_ps[b]], w=[B_QT[hl][g], B_QTx[hl]])
                              _ck("mp2")
                              rope_rows(QT[hl], 32, ps[:, b, :], 32, 32, g, B_QT[hl][g])
                              _ck("mp3")
                              b = next_pbank()
                              for kc in range(2):
                                  sc.mm(ps[:, b, :], wukk[:, kc, hl * 128:(hl + 1) * 128], ckvT[:, kc, cs], kc == 0,
                                        kc == 1, r=[B_wu, B_ckv[g]], w=[B_ps[b]])
                              sc.act(KT[hl][:, cs], ps[:, b, :], AF.Copy, r=[B_ps[b]], w=[B_KT[hl][g], B_KTx[hl]])
                              _ck("mp4")
                              sc.cp("dve", KT[hl][0:32, cs], kropeT[:, cs], r=[B_krope[g]], w=[B_KT[hl][g]])
                              _ck("mp5")
                      _ck("mlaproj")
                      for hl in range(2):
                          attention(hl, pr, 128, 96.0 ** -0.5, oTb, B_oTb)
                      flush_pv()
                  sc.barrier()
                  if dbg and s == 0:
                      for name in dbg:
                          src = {"hT": hT, "oTa": oTa, "oTb": oTb}[name]
                          for kc in range(dbg[name][0] // 128):
                              sc.dma("sp", dbg_d[name][kc * 128:(kc + 1) * 128, :], src[:, kc, :], out_sem[0], w=[])
                  sc.barrier()

              _ck("phase2")
              with ExitStack() as p3:
                  def sb3(name, shape, dt):
                      return sb(name, shape, dt, p3)

                  mergedT = sb3("mergedT", [128, 8, S], BF16)
                  gpm_bc = sb3("gpm_bc", [128, 1024], F32)
                  B_gbc = sc.buf("gbc")
                  sc.dma("sp", gpm_bc[:], gpm_d[0:1, :].partition_broadcast(128), sc.dsem("gbc"), w=[B_gbc])
                  B_mg = sc.bufs_n("mg", 4)
                  wo = sb3("wo", [128, 8, 1024], BF16)
                  B_wo = sc.buf("wo")
                  ds_wo = sc.dsem("wo")
                  wg = [sb3("wg0", [128, 8, 256], BF16), sb3("wg1", [128, 8, 256], BF16)]
                  wab = [sb3("wab0", [128, 4, 256], BF16), sb3("wab1", [128, 4, 256], BF16)]
                  B_wg = sc.bufs_n("wg", 2)
                  ds_wg = [sc.dsem("wg0"), sc.dsem("wg1")]
                  sig = [[sb3("sig%d%d" % (a, b), [128, 512], F32) for b in range(2)] for a in range(2)]
                  mt = [[sb3("mt%d%d" % (a, b), [128, 512], F32) for b in range(2)] for a in range(2)]
                  B_sig = [sc.bufs_n("sig%d_" % a, 2) for a in range(2)]
                  B_mt = [sc.bufs_n("mt%d_" % a, 2) for a in range(2)]
                  ysb = sb3("ysb", [128, 1024], F32)
                  tmpy = sb3("tmpy", [128, 1024], F32)
                  junk3 = sb3("junk3", [128, 1024], F32)
                  xt3 = [sb3("xt3_0", [128, 1024], F32), sb3("xt3_1", [128, 1024], F32)]
                  x1t = [sb3("x1t0", [128, 1024], F32), sb3("x1t1", [128, 1024], F32)]
                  st3 = sb3("st3", [128, 2], F32)
                  B_ysb = sc.buf("ysb")
                  B_tmpy = sc.buf("tmpy")
                  B_junk3 = sc.buf("junk3")
                  B_xt3 = sc.bufs_n("xt3", 2)
                  B_x1t = sc.bufs_n("x1t", 2)
                  B_st3 = sc.buf("st3")

                  def load_w3(fo, sl):
                      sc.dma("pool", wg[sl][:, :, 0:128], w_gate_d[:, :, fo * 128:(fo + 1) * 128], ds_wg[sl],
                             w=[B_wg[sl]])
                      sc.dma("pool", wg[sl][:, :, 128:256], w_gate_d[:, :, 1024 + fo * 128:1024 + (fo + 1) * 128],
                             ds_wg[sl], w=[B_wg[sl]])
                      sc.dma("pool", wab[sl][:, :, 0:128], w_a_d[:, :, fo * 128:(fo + 1) * 128], ds_wg[sl],
                             w=[B_wg[sl]])
                      sc.dma("pool", wab[sl][:, :, 128:256], w_b_d[:, :, fo * 128:(fo + 1) * 128], ds_wg[sl],
                             w=[B_wg[sl]])

                  load_w3(0, 0)
                  sc.dma("pool", wo[:], w_out_d[:, :, :], ds_wo, w=[B_wo])
                  it = 0
                  for fo in range(8):
                      sl = fo % 2
                      if fo + 1 < 8:
                          load_w3(fo + 1, 1 - sl)
                      for g in range(4):
                          cs = slice(g * 512, (g + 1) * 512)
                          a = it % 2
                          it += 1
                          bb = 4 * a
                          for half in range(2):
                              for kc in range(8):
                                  sc.mm(ps[:, bb + half, :], wg[sl][:, kc, half * 128:(half + 1) * 128], hT[:, kc, cs],
                                        kc == 0, kc == 7, r=[B_wg[sl], B_hT[g]], w=[B_ps[bb + half]])
                              sc.act(sig[a][half][:], ps[:, bb + half, :], AF.Sigmoid, r=[B_ps[bb + half], B_const],
                                     w=[B_sig[a][half]], bias=bgate[:, half * 8 + fo:half * 8 + fo + 1], scale=1.0)
                          for half in range(2):
                              oT = oTa if half == 0 else oTb
                              B_oT = B_oTa if half == 0 else B_oTb
                              for pr in range(4):
                                  sc.mm(ps[:, bb + 2 + half, :], wab[sl][:, pr, half * 128:(half + 1) * 128],
                                        oT[:, pr, cs], pr == 0, pr == 3, r=[B_wg[sl], B_oT[pr][g]],
                                        w=[B_ps[bb + 2 + half]])
                              sc.tt("dve", mt[a][half][:], ps[:, bb + 2 + half, :], sig[a][half][:], ALU.mult,
                                    r=[B_ps[bb + 2 + half], B_sig[a][half]], w=[B_mt[a][half]])
                          sc.tt("pool", mergedT[:, fo, cs], mt[a][0][:], mt[a][1][:], ALU.add,
                                r=[B_mt[a][0], B_mt[a][1]], w=[B_mg[g]])
                  for tt in range(16):
                      sl = tt % 2
                      yb = 2 * sl
                      sc.dma("sp", xt3[sl][:], x_d[r0 + tt * 128:r0 + (tt + 1) * 128, :], xt_sem[sl], w=[B_xt3[sl]])
                      for half in range(2):
                          for fo in range(8):
                              sc.mm(ps[:, yb + half, :], mergedT[:, fo, tt * 128:(tt + 1) * 128],
                                    wo[:, fo, half * 512:(half + 1) * 512], fo == 0, fo == 7, r=[B_mg[tt // 4], B_wo],
                                    w=[B_ps[yb + half]])
                      sc.act(ysb[:, 0:512], ps[:, yb, :], AF.Copy, r=[B_ps[yb]], w=[B_ysb])
                      sc.act(ysb[:, 512:1024], ps[:, yb + 1, :], AF.Copy, r=[B_ps[yb + 1]], w=[B_ysb])
                      sc.stt("dve", junk3[:], ysb[:], 1.0, ysb[:], ALU.mult, ALU.mult, r=[B_ysb], w=[B_junk3, B_st3],
                             accum_out=st3[:, 0:1])
                      sc.act(st3[:, 1:2], st3[:, 0:1], AF.Sqrt, r=[B_st3, B_const], w=[B_st3], bias=fcol[:, 4:5], scale=1.0 / D)
                      sc.op("dve", partial(nc.vector.reciprocal, st3[:, 1:2], st3[:, 1:2]), r=[B_st3], w=[B_st3])
                      sc.stt("dve", tmpy[:], ysb[:], st3[:, 1:2], gpm_bc[:], ALU.mult, ALU.mult,
                             r=[B_ysb, B_st3, B_gbc], w=[B_tmpy])
                      sc.tt("pool", x1t[sl][:], tmpy[:], xt3[sl][:], ALU.add, r=[B_tmpy, B_xt3[sl]], w=[B_x1t[sl]])
                      sc.dma("sp", out_d[r0 + tt * 128:r0 + (tt + 1) * 128, :], x1t[sl][:], out_sem[sl],
                             r=[B_x1t[sl]], w=[B_x1d[s][tt]])
                  sc.barrier()

              _ck("phase3")
              with ExitStack() as p4:
                  def sb4(name, shape, dt):
                      return sb(name, shape, dt, p4)

                  wdn = sb4("wdn", [128, 32, 1024], BF16)
                  gpl_bc = sb4("gpl_bc", [128, 1024], F32)
                  B_gbc4 = sc.buf("gbc4")
                  sc.dma("sp", gpl_bc[:], gpl_d[0:1, :].partition_broadcast(128), sc.dsem("gbc"), w=[B_gbc4])
                  B_wdn = sc.bufs_n("wdn", 4)
                  ds_wdn = sc.dsem("wdn")
                  wup = [oTa[:, 2 * i:2 * i + 2, :].rearrange("p a (k n) -> p (a k) n", n=512) for i in range(2)]
                  B_wup = sc.bufs_n("wup", 2)
                  ds_wup = [sc.dsem("wup0"), sc.dsem("wup1")]
                  def aTs(uc, lo=0, hi=512):
                      return hT[:, uc // 4, (uc % 4) * 512 + lo:(uc % 4) * 512 + hi]

                  B_aT = sc.bufs_n("aT", 32)
                  h2T = oTb[:, 0:2, :].rearrange("p a (k n) -> p (a k) n", n=512)
                  B_h2T = sc.buf("h2T")
                  x1g = [sb4("x1g%d" % i, [128, 1024], F32) for i in range(4)]
                  B_x1g = sc.bufs_n("x1g", 4)
                  xn4 = [oTb[:, 2, 0:1024], oTb[:, 2, 1024:2048]]
                  B_xn4 = sc.bufs_n("xn4", 2)
                  junk4 = sb4("junk4", [128, 1024], F32)
                  B_junk4 = sc.buf("junk4")
                  st4 = sb4("st4", [128, 4], F32)
                  B_st4 = sc.bufs_n("st4", 2)
                  rl = [sb4("rl0", [128, 512], F32), sb4("rl1", [128, 512], F32)]
                  B_rl = sc.bufs_n("rl", 2)
                  ysb4 = sb4("ysb4", [128, 1024], F32)
                  tmp4 = sb4("tmp4", [128, 1024], F32)
                  ot = [sb4("ot0", [128, 1024], F32), sb4("ot1", [128, 1024], F32)]
                  B_ysb4 = sc.buf("ysb4")
                  B_tmp4 = sc.buf("tmp4")
                  B_ot = sc.bufs_n("ot", 2)
                  ds_x1g = [sc.dsem("x1g%d" % i) for i in range(4)]

                  for kcg in range(4):
                      sc.dma("pool", wdn[:, kcg * 8:(kcg + 1) * 8, :], w_down_d[:, kcg * 8:(kcg + 1) * 8, :], ds_wdn,
                             w=[B_wdn[kcg]])
                  upcnt = 0
                  for g in range(4):
                      sc.dma("pool", wup[upcnt % 2], w_up_d[:, :, 0:512], ds_wup[upcnt % 2], w=[B_wup[upcnt % 2]])
                      for j in range(4):
                          tt = g * 4 + j
                          sc.dma("sp", x1g[j][:], out_d[r0 + tt * 128:r0 + (tt + 1) * 128, :], ds_x1g[j],
                                 r=[B_x1d[s][tt]], w=[B_x1g[j]])
                          sl = j % 2
                          norm_transpose(x1g[j][:], B_x1g[j], j * 128, xn4[sl], B_xn4[sl], st4[:, sl:sl + 1],
                                         st4[:, 2 + sl:3 + sl], B_st4[sl], junk4, B_junk4, sl, gpre2, h2T, B_h2T)
                      for ucg in range(8):
                          sl = upcnt % 2
                          upcnt += 1
                          if ucg + 1 < 8:
                              sc.dma("pool", wup[1 - sl], w_up_d[:, :, (ucg + 1) * 512:(ucg + 2) * 512],
                                     ds_wup[1 - sl], w=[B_wup[1 - sl]])
                          for ucl in range(4):
                              uc = ucg * 4 + ucl
                              b = 2 + (uc % 2)
                              for kc in range(8):
                                  sc.mm(ps[:, b, :], wup[sl][:, kc, ucl * 128:(ucl + 1) * 128], h2T[:, kc, :], kc == 0,
                                        kc == 7, r=[B_wup[sl], B_h2T], w=[B_ps[b]])
                              sc.act(rl[uc % 2][:], ps[:, b, :], AF.Relu, r=[B_ps[b]], w=[B_rl[uc % 2]])
                              sc.tt("pool", aTs(uc), rl[uc % 2][:], rl[uc % 2][:], ALU.mult, r=[B_rl[uc % 2]],
                                    w=[B_aT[uc]])
                      if g + 1 < 4:
                          pass
                      for j in range(4):
                          tt = g * 4 + j
                          yb = 4 + 2 * (j % 2)
                          for half in range(2):
                              for uc in range(32):
                                  sc.mm(ps[:, yb + half, :], aTs(uc, j * 128, (j + 1) * 128),
                                        wdn[:, uc, half * 512:(half + 1) * 512], uc == 0, uc == 31,
                                        r=[B_aT[uc], B_wdn[uc // 8]], w=[B_ps[yb + half]])
                          sc.act(ysb4[:, 0:512], ps[:, yb, :], AF.Copy, r=[B_ps[yb]], w=[B_ysb4])
                          sc.act(ysb4[:, 512:1024], ps[:, yb + 1, :], AF.Copy, r=[B_ps[yb + 1]], w=[B_ysb4])
                          sc.stt("dve", junk4[:], ysb4[:], 1.0, ysb4[:], ALU.mult, ALU.mult, r=[B_ysb4],
                                 w=[B_junk4, B_st4[0]], accum_out=st4[:, 0:1])
                          sc.act(st4[:, 2:3], st4[:, 0:1], AF.Sqrt, r=[B_st4[0], B_const], w=[B_st4[0]], bias=fcol[:, 4:5], scale=1.0 / D)
                          sc.op("dve", partial(nc.vector.reciprocal, st4[:, 2:3], st4[:, 2:3]), r=[B_st4[0]], w=[B_st4[0]])
                          sc.stt("dve", tmp4[:], ysb4[:], st4[:, 2:3], gpl_bc[:], ALU.mult, ALU.mult,
                                 r=[B_ysb4, B_st4[0], B_gbc4], w=[B_tmp4])
                          sc.tt("pool", ot[j % 2][:], tmp4[:], x1g[j][:], ALU.add, r=[B_tmp4, B_x1g[j]], w=[B_ot[j % 2]])
                          sc.dma("sp", out_d[r0 + tt * 128:r0 + (tt + 1) * 128, :], ot[j % 2][:], out_sem[j % 2],
                                 r=[B_ot[j % 2]], w=[B_x1d[s][tt]])
                  sc.barrier()

        except _Stop:
            raise
        n_ins, n_wait = sc.emit()
        build_nc.stats = (n_ins, n_wait)
    return nc


_NC_CACHE = {}


def kernel(x, positions, g_pre_mix, w_in, b_gate, g_q_norm, w_uq, g_kv_norm, w_ukv, w_branch_a, w_branch_b, w_out,
           g_post_mix, g_pre_mlp, w_up, w_down, g_post_mlp):
    x = np.asarray(x, dtype=np.float32)
    positions = np.asarray(positions, dtype=np.int32)
    args = [np.asarray(a, dtype=np.float32) for a in
            (w_in, w_uq, w_ukv, w_branch_a, w_branch_b, w_out, w_up, w_down, g_pre_mix, b_gate, g_q_norm,
             g_kv_norm, g_post_mix, g_pre_mlp, g_post_mlp)]
    shared = _host_weights(*args)
    shared.update(_host_consts())
    if "nc" not in _NC_CACHE:
        _NC_CACHE["nc"] = build_nc(NSEQ)
    nc = _NC_CACHE["nc"]
    in_maps = []
    for c in range(NCORES):
        m = dict(shared)
        m["x"] = np.ascontiguousarray(x[c * NSEQ:(c + 1) * NSEQ].reshape(NSEQ * S, D))
        m["pos"] = np.ascontiguousarray(positions[c * NSEQ:(c + 1) * NSEQ])
        in_maps.append(m)
    res = run_bass_kernel_spmd(nc, in_maps, core_ids=list(range(NCORES)))
    out = np.concatenate([np.asarray(r["out"]).reshape(NSEQ, S, D) for r in res.results], axis=0)
    return out.astype(np.float32, copy=False)
```

```python
import math
from contextlib import ExitStack
from functools import partial

import numpy as np
import concourse.bass as bass
import concourse.mybir as mybir
from concourse.bass_utils import run_bass_kernel_spmd

F32 = mybir.dt.float32
BF16 = mybir.dt.bfloat16
I32 = mybir.dt.int32
ALU = mybir.AluOpType
AF = mybir.ActivationFunctionType
AX = mybir.AxisListType

NCORES = 8
NSEQ = 4
S = 2048
D = 1024
EPS = 1e-6
NEG = -30000.0
PIPE = 3
THETA = 500000.0


class _Stop(Exception):
    pass


_STOP = [None, None, None]


def _ck(name):
    if _STOP[0] == name:
        sc = _STOP[1]
        sc.barrier()
        build_nc.stats = sc.emit()
        raise _Stop()


class Buf:
    __slots__ = ("name", "last_w", "readers")

    def __init__(self, name):
        self.name = name
        self.last_w = None
        self.readers = []


class DSem:
    __slots__ = ("h", "count", "last", "name")

    def __init__(self, h, name=""):
        self.name = name
        self.h = h
        self.count = 0
        self.last = None


class Ins:
    __slots__ = ("eng", "fn", "deps", "sig", "sigval", "dsem", "dval", "is_dma")

    def __init__(self, eng, fn):
        self.eng = eng
        self.fn = fn
        self.deps = []
        self.sig = False
        self.sigval = 0
        self.dsem = None
        self.dval = 0
        self.is_dma = False


class Sched:
    ENGS = ("pe", "act", "dve", "pool", "sp")

    def __init__(self, nc, stack):
        self.nc = nc
        self.stack = stack
        self.eobj = {"pe": nc.tensor, "act": nc.scalar, "dve": nc.vector, "pool": nc.gpsimd, "sp": nc.sync}
        self.esem = {e: stack.enter_context(nc.semaphore("es_" + e)) for e in self.ENGS}
        self.prog = []
        self.last = {e: None for e in self.ENGS}
        self.lastc = {e: None for e in self.ENGS}
        self.bufs = []
        self.dsems = []
        self._dsc = {}

    def buf(self, name):
        b = Buf(name)
        self.bufs.append(b)
        return b

    def bufs_n(self, name, n):
        return [self.buf("%s%d" % (name, i)) for i in range(n)]

    def dsem(self, name):
        name = name.split("%")[0]
        if name in self._dsc:
            return self._dsc[name]
        d = self._dsc[name] = DSem(self.stack.enter_context(self.nc.semaphore("ds_" + name)), name)
        self.dsems.append(d)
        return d

    @staticmethod
    def _need(src, dst, kind):
        if src.is_dma or dst.is_dma:
            return True
        if src.eng != dst.eng:
            return True
        if src.eng == "pe":
            return False
        return True

    def _track(self, ins, r, w):
        deps = ins.deps

        def add(d):
            if d.is_dma:
                deps.append((d.dsem, d.dsem.count))
            else:
                d.sig = True
                deps.append(d)

        for b in r:
            lw = b.last_w
            if lw is not None and self._need(lw, ins, "RAW"):
                add(lw)
        for b in w:
            lw = b.last_w
            if lw is not None and self._need(lw, ins, "WAW"):
                add(lw)
            for rd in b.readers:
                if rd is not ins and self._need(rd, ins, "WAR"):
                    add(rd)
        for b in w:
            b.last_w = ins
            b.readers = []
        for b in r:
            if b.last_w is ins:
                continue
            if not ins.is_dma:
                b.readers = [x for x in b.readers if x.is_dma or x.eng != ins.eng]
            b.readers.append(ins)
        self.prog.append(ins)
        self.last[ins.eng] = ins
        if not ins.is_dma:
            self.lastc[ins.eng] = ins

    def op(self, eng, fn, r=(), w=()):
        ins = Ins(eng, fn)
        self._track(ins, r, w)
        return ins

    def dma(self, eng, out, in_, ds, r=(), w=()):
        ins = Ins(eng, partial(self.eobj[eng].dma_start, out=out, in_=in_))
        if not ds.name.endswith("@" + eng):
            ds = self.dsem(ds.name.split("@")[0] + "@" + eng)
        ins.is_dma = True
        ins.dsem = ds
        self._track(ins, r, w)
        ds.count += 16
        ins.dval = ds.count
        ds.last = ins
        return ins

    def barrier(self):
        lasts = [self.lastc[e] for e in self.ENGS if self.lastc[e] is not None]
        dl = [d.last for d in self.dsems if d.last is not None]
        for e in self.ENGS:
            ins = Ins(e, None)
            for l in lasts:
                if l.eng != e:
                    ins.deps.append(l)
                    l.sig = True
            ins.deps.extend((d.dsem, d.dsem.count) for d in dl)
            self.prog.append(ins)
        for b in self.bufs:
            b.last_w = None
            b.readers = []

    def emit(self):
        cnt = {e: 0 for e in self.ENGS}
        for ins in self.prog:
            if ins.sig and not ins.is_dma and ins.fn is not None:
                cnt[ins.eng] += 1
                ins.sigval = cnt[ins.eng]
        waited = {}
        nwait = 0
        for ins in self.prog:
            E = self.eobj[ins.eng]
            for d in ins.deps:
                if isinstance(d, tuple):
                    sem, val = d[0].h, d[1]
                else:
                    sem, val = self.esem[d.eng], d.sigval
                key = (ins.eng, id(sem))
                if waited.get(key, 0) >= val:
                    continue
                waited[key] = val
                E.wait_ge(sem, val)
                nwait += 1
            if ins.fn is None:
                continue
            bi = ins.fn()
            if ins.is_dma:
                bi.then_inc(ins.dsem.h, 16)
            elif ins.sig:
                bi.then_inc(self.esem[ins.eng], 1)
        return len(self.prog), nwait

    def mm(self, out, lhsT, rhs, start, stop, r, w):
        return self.op("pe", partial(self.nc.tensor.matmul, out, lhsT, rhs, start=start, stop=stop), r, w)

    def tr(self, out, in_, ident, r, w):
        return self.op("pe", partial(self.nc.tensor.transpose, out, in_, ident), r, w)

    def act(self, out, in_, func, r, w, **kw):
        return self.op("act", partial(self.nc.scalar.activation, out, in_, func, **kw), r, w)

    def tt(self, eng, out, in0, in1, op, r, w):
        return self.op(eng, partial(self.eobj[eng].tensor_tensor, out=out, in0=in0, in1=in1, op=op), r, w)

    def ts(self, eng, out, in0, s1, s2, op0, op1, r, w):
        if op1 is None:
            fn = partial(self.eobj[eng].tensor_scalar, out, in0, s1, None, op0)
        else:
            fn = partial(self.eobj[eng].tensor_scalar, out, in0, s1, s2, op0, op1)
        return self.op(eng, fn, r, w)

    def stt(self, eng, out, in0, scalar, in1, op0, op1, r, w, accum_out=None):
        if accum_out is None:
            fn = partial(self.eobj[eng].scalar_tensor_tensor, out, in0, scalar, in1, op0, op1)
        else:
            fn = partial(self.eobj[eng].scalar_tensor_tensor, out, in0, scalar, in1, op0, op1, accum_out)
        return self.op(eng, fn, r, w)

    def cp(self, eng, out, in_, r, w):
        return self.op(eng, partial(self.eobj[eng].tensor_copy, out, in_), r, w)

    def memset(self, eng, ap, val, r, w):
        return self.op(eng, partial(self.eobj[eng].memset, ap, val), r, w)


def _host_consts():
    c = {}
    c["ident"] = np.eye(128, dtype=np.float32)
    p = np.arange(128)[:, None]
    q = np.arange(128)[None, :]
    c["tri"] = np.where(p <= q, 0.0, NEG).astype(np.float32)
    k = np.arange(S)
    n = np.arange(8)
    c["kind"] = (k[None, :] // 256 == n[:, None]).astype(np.float32)
    c["qbias0"] = np.where(n[:, None] <= (k[None, :1024] // 256), 0.0, NEG).astype(np.float32)
    own = (np.arange(8, 16) // 2)
    mn = np.zeros((8, 2, 8), np.float32)
    mm_ = np.zeros((8, 2, 8), np.float32)
    for i in range(8):
        for nn in range(8):
            if nn == own[i]:
                mn[i, :, nn] = 1e30
            elif nn > own[i]:
                mn[i, :, nn] = -1e30
            if nn >= own[i]:
                mm_[i, :, nn] = -1e30
    c["mask_n"] = np.broadcast_to(mn.reshape(1, 128), (128, 128)).copy()
    c["mask_m"] = np.broadcast_to(mm_.reshape(1, 128), (128, 128)).copy()
    fcol = np.zeros((128, 8), np.float32)
    invm = THETA ** (-np.arange(8, dtype=np.float32) * (2.0 / 16))
    invl = THETA ** (-np.arange(16, dtype=np.float32) * (2.0 / 32))
    fcol[:, 0] = 1.0 / (2 * math.pi)
    fcol[:, 1] = 0.0
    for j in range(16):
        fcol[j, 0] = invm[j % 8] / (2 * math.pi)
        fcol[j, 1] = 0.5 if j < 8 else 0.0
    for j in range(32):
        fcol[32 + j, 0] = invl[j % 16] / (2 * math.pi)
        fcol[32 + j, 1] = 0.5 if j < 16 else 0.0
    fcol[:, 2] = 0.25
    fcol[:, 4] = EPS
    c["fcol"] = fcol
    return c


def _host_weights(w_in, w_uq, w_ukv, w_branch_a, w_branch_b, w_out, w_up, w_down, g_pre_mix, b_gate,
                  g_q_norm, g_kv_norm, g_post_mix, g_pre_mlp, g_post_mlp):
    w = {}
    wi = w_in[0]
    cols = []
    for pr in range(4):
        for base0 in (0, 512):
            for h in (2 * pr, 2 * pr + 1):
                b = base0 + h * 64
                cols.extend(range(b, b + 64))
                cols.extend(range(b + 8, b + 16))
                cols.extend(range(b, b + 8))
    w["w_qk"] = np.ascontiguousarray(wi[:, cols])
    w["w_v"] = np.ascontiguousarray(wi[:, 1024:1536])
    w["w_cq"] = np.ascontiguousarray(wi[:, 1536:1920])
    w["w_ckv"] = np.ascontiguousarray(wi[:, 1920:2176])
    kr = list(range(2176, 2208)) + list(range(2176 + 16, 2208)) + list(range(2176, 2176 + 16))
    w["w_kr"] = np.ascontiguousarray(wi[:, kr])
    w["w_gate"] = np.ascontiguousarray(wi[:, 2208:4256])
    uq = w_uq[0]
    cols = []
    for h in range(8):
        b = h * 96
        cols.extend(range(b + 64, b + 96))
        cols.extend(range(b + 80, b + 96))
        cols.extend(range(b + 64, b + 80))
        cols.extend(range(b, b + 64))
    w["w_uq"] = np.ascontiguousarray(uq[:, cols])
    ukv = w_ukv[0]
    wk = np.zeros((256, 8, 128), np.float32)
    wv = np.zeros((256, 8, 64), np.float32)
    for h in range(8):
        wk[:, h, 64:128] = ukv[:, h * 128:h * 128 + 64]
        wv[:, h, :] = ukv[:, h * 128 + 64:h * 128 + 128]
    w["w_ukvk"] = wk.reshape(256, 1024)
    w["w_ukvv"] = wv.reshape(256, 512)
    w["w_a"] = np.ascontiguousarray(w_branch_a[0])
    w["w_b"] = np.ascontiguousarray(w_branch_b[0])
    w["w_out"] = np.ascontiguousarray(w_out[0])
    w["w_up"] = np.ascontiguousarray(w_up[0])
    w["w_down"] = np.ascontiguousarray(w_down[0])

    def colsT(v, nk):
        return np.ascontiguousarray(v.reshape(nk, 128).T)

    w["gpre_T"] = colsT(g_pre_mix[0], 8)
    w["gpre2_T"] = colsT(g_pre_mlp[0], 8)
    w["gq_T"] = colsT(g_q_norm[0], 3)
    w["gkv_T"] = colsT(g_kv_norm[0], 2)
    w["bgate_T"] = colsT(b_gate[0], 16)
    w["gpm"] = np.ascontiguousarray(g_post_mix[0].reshape(1, 1024))
    w["gpl"] = np.ascontiguousarray(g_post_mlp[0].reshape(1, 1024))
    return {k: np.asarray(v, dtype=np.float32) for k, v in w.items()}


def build_nc(nseq=NSEQ, dbg=None):
    try:
        return _build_nc(nseq, dbg)
    except _Stop:
        return _STOP[2]


def _build_nc(nseq=NSEQ, dbg=None):
    nc = bass.Bass("TRN2", target_bir_lowering=False)
    _STOP[2] = nc
    NTOK = nseq * S

    def din(name, shape, dt=F32):
        return nc.dram_tensor(name, list(shape), dt, kind="ExternalInput").ap()

    x_d = din("x", [NTOK, D])
    pos_d = din("pos", [nseq, S], I32)
    w_qk_d = din("w_qk", [1024, 1280]).rearrange("(kc p) n -> p kc n", p=128)
    w_v_d = din("w_v", [1024, 512]).rearrange("(kc p) n -> p kc n", p=128)
    w_cq_d = din("w_cq", [1024, 384]).rearrange("(kc p) n -> p kc n", p=128)
    w_ckv_d = din("w_ckv", [1024, 256]).rearrange("(kc p) n -> p kc n", p=128)
    w_kr_d = din("w_kr", [1024, 64]).rearrange("(kc p) n -> p kc n", p=128)
    w_gate_d = din("w_gate", [1024, 2048]).rearrange("(kc p) n -> p kc n", p=128)
    w_uq_d = din("w_uq", [384, 1024]).rearrange("(kc p) n -> p kc n", p=128)
    w_ukvk_d = din("w_ukvk", [256, 1024]).rearrange("(kc p) n -> p kc n", p=128)
    w_ukvv_d = din("w_ukvv", [256, 512]).rearrange("(kc p) n -> p kc n", p=128)
    w_a_d = din("w_a", [512, 1024]).rearrange("(kc p) n -> p kc n", p=128)
    w_b_d = din("w_b", [512, 1024]).rearrange("(kc p) n -> p kc n", p=128)
    w_out_d = din("w_out", [1024, 1024]).rearrange("(kc p) n -> p kc n", p=128)
    w_up_d = din("w_up", [1024, 4096]).rearrange("(kc p) n -> p kc n", p=128)
    w_down_d = din("w_down", [4096, 1024]).rearrange("(kc p) n -> p kc n", p=128)
    gpre_d = din("gpre_T", [128, 8])
    gpre2_d = din("gpre2_T", [128, 8])
    gq_d = din("gq_T", [128, 3])
    gkv_d = din("gkv_T", [128, 2])
    bgate_d = din("bgate_T", [128, 16])
    gpm_d = din("gpm", [1, 1024])
    gpl_d = din("gpl", [1, 1024])
    ident_d = din("ident", [128, 128])
    tri_d = din("tri", [128, 128])
    kind_d = din("kind", [8, S])
    qbias0_d = din("qbias0", [8, 1024])
    maskn_d = din("mask_n", [128, 128])
    maskm_d = din("mask_m", [128, 128])
    fcol_d = din("fcol", [128, 8])
    out_d = nc.dram_tensor("out", [NTOK, D], F32, kind="ExternalOutput").ap()
    dbg_d = {}
    if dbg:
        for name, shape in dbg.items():
            dbg_d[name] = nc.dram_tensor("dbg_" + name, list(shape), BF16, kind="ExternalOutput").ap()

    with ExitStack() as st:
        sc = Sched(nc, st)
        _STOP[1] = sc

        uniq = [0]

        def sb(name, shape, dt, stack=st):
            uniq[0] += 1
            return stack.enter_context(nc.sbuf_tensor("s%d_%s" % (uniq[0], name), list(shape), dt))

        ps = st.enter_context(nc.psum_tensor("psum_all", [128, 8, 512], F32))
        psb = ps.bitcast(BF16) if hasattr(ps, "bitcast") else None
        B_ps = sc.bufs_n("ps", 8)

        ident = sb("ident", [128, 128], BF16)
        tri = sb("tri", [128, 128], BF16)
        gpre = sb("gpre", [128, 8], F32)
        gpre2 = sb("gpre2", [128, 8], F32)
        gq = sb("gq", [128, 3], F32)
        gkv = sb("gkv", [128, 2], F32)
        bgate = sb("bgate", [128, 16], F32)
        fcol = sb("fcol", [128, 8], F32)
        ones_bf = sb("ones_bf", [128, 128], BF16)
        hT = sb("hT", [128, 8, S], BF16)
        oTa = sb("oTa", [128, 4, S], BF16)
        oTb = sb("oTb", [128, 4, S], BF16)
        B_const = sc.buf("const")
        B_hT = sc.bufs_n("hT", 4)
        B_oTa = [sc.bufs_n("oTa%d_" % p, 4) for p in range(4)]
        B_oTb = [sc.bufs_n("oTb%d_" % p, 4) for p in range(4)]
        ds_const = sc.dsem("const")

        for (t, d) in ((gpre, gpre_d), (gpre2, gpre2_d), (gq, gq_d), (gkv, gkv_d), (bgate, bgate_d),
                       (fcol, fcol_d)):
            sc.dma("sp", t[:], d[:, :], ds_const, w=[B_const])
        sc.dma("pool", ident[:], ident_d[:, :], ds_const, w=[B_const])
        sc.dma("pool", tri[:], tri_d[:, :], ds_const, w=[B_const])
        sc.memset("dve", ones_bf[:], 1.0, r=[], w=[B_const])
        sc.barrier()

        xt_sem = [sc.dsem("xt0"), sc.dsem("xt1")]
        out_sem = [sc.dsem("o0"), sc.dsem("o1")]
        B_x1d = [sc.bufs_n("x1d%d_" % s, 16) for s in range(nseq)]

        def norm_transpose(src_tile, B_src, tt_col, xn, B_xn, ssq, rstd, B_st, junk, B_junk, bank, gcol, dstT,
                           B_dst):
            sc.stt("dve", junk[:], src_tile, 1.0, src_tile, ALU.mult, ALU.mult, r=[B_src], w=[B_junk, B_st],
                   accum_out=ssq[:, 0:1])
            sc.act(rstd[:, 0:1], ssq[:, 0:1], AF.Sqrt, r=[B_st, B_const], w=[B_st], bias=fcol[:, 4:5], scale=1.0 / D)
            sc.op("dve", partial(nc.vector.reciprocal, rstd[:, 0:1], rstd[:, 0:1]), r=[B_st], w=[B_st])
            sc.ts("dve", xn[:], src_tile, rstd[:, 0:1], None, ALU.mult, None, r=[B_src, B_st], w=[B_xn])
            pT = psb[:, bank, :]
            for kc in range(8):
                sc.tr(pT[:, kc * 128:(kc + 1) * 128], xn[:, kc * 128:(kc + 1) * 128], ident[:], r=[B_xn, B_const],
                      w=[B_ps[bank]])
            sc.tt("dve", dstT[:, :, tt_col:tt_col + 128], pT.rearrange("p (k t) -> p k t", k=8),
                  gcol[:, :].unsqueeze(2).broadcast_to([128, 8, 128]), ALU.mult, r=[B_ps[bank], B_const],
                  w=[B_dst])

        try:
          for s in range(nseq):
              r0 = s * S
              with ExitStack() as p2:
                  def sb2(name, shape, dt):
                      return sb(name, shape, dt, p2)

                  COS = sb2("COS", [128, S], F32)
                  SIN = sb2("SIN", [128, S], F32)
                  p1 = p2.enter_context(ExitStack())

                  def sb1(name, shape, dt):
                      return sb(name, shape, dt, p1)

                  xt = [sb1("xt0", [128, 1024], F32), sb1("xt1", [128, 1024], F32)]
                  xn = [sb1("xn0", [128, 1024], BF16), sb1("xn1", [128, 1024], BF16)]
                  junk = sb1("junk", [128, 1024], F32)
                  ssq = sb1("ssq", [128, 2], F32)
                  rstd = sb1("rstd", [128, 2], F32)
                  B_xt = sc.bufs_n("xt", 2)
                  B_xn = sc.bufs_n("xn", 2)
                  B_junk = sc.buf("junk")
                  B_st = sc.bufs_n("st", 2)
                  posi = sb1("posi", [128, S], I32)
                  B_tab = sc.buf("tab")
                  B_posi = sc.buf("posi")
                  ds_pos = sc.dsem("pos")

                  sc.dma("sp", posi[:], pos_d[s:s + 1, :].partition_broadcast(128), ds_pos, w=[B_posi])
                  kint = sb1("kint", [128, 1024], I32)
                  kflt = sb1("kflt", [128, 1024], F32)
                  B_kint = sc.buf("kint")
                  for hh in range(2):
                      cs = slice(hh * 1024, (hh + 1) * 1024)
                      sc.cp("dve", junk[:, :], posi[:, cs], r=[B_posi], w=[B_junk])
                      for (tab, phc) in ((SIN, 1), (COS, 2)):
                          sc.ts("dve", tab[:, cs], junk[:, :], fcol[:, 0:1], fcol[:, phc:phc + 1], ALU.mult, ALU.add,
                                r=[B_junk, B_const], w=[B_tab])
                          sc.cp("dve", kint[:, :], tab[:, cs], r=[B_tab], w=[B_kint])
                          sc.cp("dve", kflt[:, :], kint[:, :], r=[B_kint], w=[B_kint])
                          sc.tt("dve", tab[:, cs], tab[:, cs], kflt[:, :], ALU.subtract, r=[B_tab, B_kint], w=[B_tab])
                          sc.act(tab[:, cs], tab[:, cs], AF.Sin, r=[B_tab], w=[B_tab], scale=6.2831845)

                  _ck("tables")
                  for tt in range(16):
                      sl = tt % 2
                      sc.dma("sp", xt[sl][:], x_d[r0 + tt * 128:r0 + (tt + 1) * 128, :], xt_sem[sl], w=[B_xt[sl]])
                      norm_transpose(xt[sl][:], B_xt[sl], tt * 128, xn[sl], B_xn[sl], ssq[:, sl:sl + 1],
                                     rstd[:, sl:sl + 1], B_st[sl], junk, B_junk, sl, gpre, hT, B_hT[tt // 4])

                  sc.barrier()
                  p1.close()

                  _ck("phase1")
                  QT = [sb2("QT0", [128, S], BF16), sb2("QT1", [128, S], BF16)]
                  KT = [sb2("KT0", [128, S], BF16), sb2("KT1", [128, S], BF16)]
                  B_QT = [sc.bufs_n("QT%d_" % i, 4) for i in range(2)]
                  B_KT = [sc.bufs_n("KT%d_" % i, 4) for i in range(2)]
                  B_QTx = sc.bufs_n("QTx", 2)
                  B_KTx = sc.bufs_n("KTx", 2)
                  Vaug = sb2("Vaug", [128, 16, 4, 192], BF16)
                  B_V = sc.bufs_n("V", 16)
                  B_Vones = sc.buf("Vones")
                  PT = [sb2("PT%d" % i, [128, 512], BF16) for i in range(4)]
                  B_PT = sc.bufs_n("PT", 4)
                  rtmp = [sb2("rtmp%d" % i, [128, 512], F32) for i in range(2)]
                  B_rtmp = sc.bufs_n("rtmp", 2)
                  t1 = sb2("t1", [128, 512], F32)
                  t2 = sb2("t2", [128, 512], F32)
                  B_t1 = sc.buf("t1")
                  B_t2 = sc.buf("t2")
                  B_wv = sc.buf("wv")
                  ds_wv = sc.dsem("wv")
                  pm = p2.enter_context(ExitStack())

                  def sb2m(name, shape, dt):
                      return sb(name, shape, dt, pm)

                  wv = sb2m("wv", [128, 8, 512], BF16)

                  wqk = [sb2m("wqk0", [128, 8, 320], BF16), sb2m("wqk1", [128, 8, 320], BF16)]
                  B_wqk = sc.bufs_n("wqk", 2)
                  ds_wqk = [sc.dsem("wqk0"), sc.dsem("wqk1")]
                  kmT = sb2m("kmT", [128, 2, 8], F32)
                  kmTb = sb2m("kmTb", [128, 2, 8], BF16)
                  B_km = sc.bufs_n("km", 2)
                  gn = sb2m("gn", [128, 128], F32)
                  gm = sb2m("gm", [128, 128], F32)
                  rk = sb2m("rk", [128, 128], F32)
                  cmpt = sb2m("cmpt", [128, 128], F32)
                  maskn = sb2m("maskn", [128, 128], F32)
                  maskm = sb2m("maskm", [128, 128], F32)
                  bpad = sb2m("bpad", [128, 8, 2, 72], BF16)
                  B_g = sc.buf("gating")
                  B_bpad = sc.buf("bpad")
                  ds_misc = sc.dsem("misc")
                  sc.dma("sp", maskn[:], maskn_d[:, :], ds_misc, w=[B_g])
                  sc.dma("sp", maskm[:], maskm_d[:, :], ds_misc, w=[B_g])
                  sc.memset("pool", bpad[:], 0.0, r=[], w=[B_bpad])
                  sc.memset("pool", Vaug[:, :, :, 64:128], 1.0, r=[], w=[B_Vones])

                  state = {"sb": 0, "pt": 0, "acc": 0, "pendq": [], "rt": 0}

                  def flush_pv(keep=0):
                      while len(state["pendq"]) > keep:
                          _flush_one(state["pendq"].pop(0))

                  def _flush_one(pd):
                      (hl, pr, kt, c0, ptb, acc, first, last, qi, oT, B_oT) = pd
                      sc.mm(ps[:, acc, c0:512], Vaug[:, kt, pr, hl * 64:hl * 64 + 128], PT[ptb][:, c0:512],
                            first, last, r=[B_V[kt], B_Vones, B_PT[ptb]], w=[B_ps[acc]])
                      if last:
                          ri = state["rt"]
                          state["rt"] = 1 - ri
                          o_lo, d_lo = (0, 64) if hl == 0 else (64, 0)
                          sc.op("dve", partial(nc.vector.reciprocal, rtmp[ri][o_lo:o_lo + 64, :],
                                               ps[d_lo:d_lo + 64, acc, :]), r=[B_ps[acc]], w=[B_rtmp[ri]])
                          sc.tt("dve", oT[o_lo:o_lo + 64, pr, qi * 512:(qi + 1) * 512], ps[o_lo:o_lo + 64, acc, :],
                                rtmp[ri][o_lo:o_lo + 64, :], ALU.mult, r=[B_ps[acc], B_rtmp[ri]], w=[B_oT[pr][qi]])

                  def attention(hl, pr, R, scale, oT, B_oT):
                      for qi in range(4):
                          q0 = qi * 512
                          nkt = 4 * qi + 4
                          acc = 5 + state["acc"]
                          state["acc"] = 1 - state["acc"]
                          for kt in range(nkt):
                              rdiag = kt - 4 * qi
                              c0 = 128 * rdiag if rdiag > 0 else 0
                              sbk = (2, 3, 4, 0)[state["sb"]]
                              state["sb"] = (state["sb"] + 1) % 4
                              ptb = state["pt"]
                              state["pt"] = (state["pt"] + 1) % 4
                              sc.mm(ps[:, sbk, c0:512], KT[hl][0:R, kt * 128:(kt + 1) * 128],
                                    QT[hl][0:R, q0 + c0:q0 + 512], True, rdiag < 0,
                                    r=[B_KT[hl][kt // 4], B_KTx[hl], B_QT[hl][qi], B_QTx[hl]], w=[B_ps[sbk]])
                              if rdiag >= 0:
                                  sc.mm(ps[:, sbk, c0:c0 + 128], ident[:], tri[:], False, True, r=[B_const],
                                        w=[B_ps[sbk]])
                              sc.act(PT[ptb][:, c0:512], ps[:, sbk, c0:512], AF.Exp, r=[B_ps[sbk]], w=[B_PT[ptb]],
                                     scale=scale)
                              flush_pv(keep=PIPE - 1)
                              state["pendq"].append((hl, pr, kt, c0, ptb, acc, kt == 0, kt == nkt - 1, qi, oT, B_oT))

                  def rope_rows(dst, nrow, pst, swap_lo, cos_lo, g, B_dst):
                      cs = slice(g * 512, (g + 1) * 512)
                      sc.tt("dve", t1[0:nrow, :], pst[swap_lo:swap_lo + nrow, :], SIN[cos_lo:cos_lo + nrow, cs],
                            ALU.mult, r=[B_ps_cur[0], B_tab, B_dst], w=[B_t1])
                      sc.tt("dve", t2[0:nrow, :], pst[0:nrow, :], COS[cos_lo:cos_lo + nrow, cs], ALU.mult,
                            r=[B_ps_cur[0], B_tab, B_dst], w=[B_t2])
                      sc.tt("dve", dst[0:nrow, cs], t1[0:nrow, :], t2[0:nrow, :], ALU.add, r=[B_t1, B_t2],
                            w=[B_dst])

                  B_ps_cur = [None]
                  pbank = {"i": 0}

                  def next_pbank():
                      b = pbank["i"]
                      pbank["i"] = 1 - b
                      B_ps_cur[0] = B_ps[b]
                      return b

                  sc.dma("pool", wv[:], w_v_d[:, :, :], ds_wv, w=[B_wv])
                  sc.dma("pool", wqk[0][:], w_qk_d[:, :, 0:320], ds_wqk[0], w=[B_wqk[0]])
                  _ck("p2init")
                  for tt in range(16):
                      b = next_pbank()
                      for kc in range(8):
                          sc.mm(ps[:, b, :], hT[:, kc, tt * 128:(tt + 1) * 128], wv[:, kc, :], kc == 0, kc == 7,
                                r=[B_hT[tt // 4], B_wv], w=[B_ps[b]])
                      src = ps[:, b, :].rearrange("p (a e d) -> p a e d", a=4, e=2)
                      sc.act(Vaug[:, tt, :, 0:64], src[:, :, 0, :], AF.Copy, r=[B_ps[b]], w=[B_V[tt]])
                      sc.cp("dve", Vaug[:, tt, :, 128:192], src[:, :, 1, :], r=[B_ps[b]], w=[B_V[tt]])
                  for pr in range(4):
                      wsl = pr % 2
                      if pr + 1 < 4:
                          sc.dma("pool", wqk[1 - wsl][:], w_qk_d[:, :, (pr + 1) * 320:(pr + 2) * 320], ds_wqk[1 - wsl],
                                 w=[B_wqk[1 - wsl]])
                      for hl in (range(2) if pr == 0 else ()):
                          sc.dma("pool", KT[hl][64:72, :], kind_d[:, :], ds_misc, w=[B_KTx[hl]])
                          sc.dma("pool", QT[hl][64:72, 0:1024], qbias0_d[:, :], ds_misc, w=[B_QTx[hl]])
                      _ck("mobaV")
                      for j in range(4):
                          hl = j % 2
                          dst = QT[hl] if j < 2 else KT[hl]
                          B_dst = B_QT[hl] if j < 2 else B_KT[hl]
                          for g in range(4):
                              b = next_pbank()
                              for kc in range(8):
                                  sc.mm(ps[0:80, b, :], wqk[wsl][:, kc, j * 80:(j + 1) * 80],
                                        hT[:, kc, g * 512:(g + 1) * 512], kc == 0, kc == 7,
                                        r=[B_wqk[wsl], B_hT[g]], w=[B_ps[b]])
                              sc.act(dst[0:64, g * 512:(g + 1) * 512], ps[0:64, b, :], AF.Copy, r=[B_ps[b]],
                                     w=[B_dst[g]])
                              rope_rows(dst, 16, ps[:, b, :], 64, 0, g, B_dst[g])
                      _ck("mobaproj")
                      for hl in range(2):
                          sc.op("dve", partial(nc.vector.tensor_reduce, kmT[0:64, hl, :],
                                               KT[hl][0:64, :].rearrange("p (n k) -> p n k", n=8), AX.X, ALU.add),
                                r=B_KT[hl], w=[B_km[hl]])
                          sc.ts("dve", kmTb[0:64, hl, :], kmT[0:64, hl, :], 1.0 / 256, None, ALU.mult, None,
                                r=[B_km[hl]], w=[B_km[hl]])
                      gb = 7
                      for qt in range(8):
                          for hl in range(2):
                              sc.mm(ps[:, gb, qt * 16 + hl * 8:qt * 16 + hl * 8 + 8],
                                    QT[hl][0:64, (8 + qt) * 128:(9 + qt) * 128], kmTb[0:64, hl, :], True, True,
                                    r=[B_QT[hl][2 + qt // 4], B_km[hl]], w=[B_ps[gb]])
                      sc.tt("dve", gn[:], ps[:, gb, 0:128], maskn[:], ALU.add, r=[B_ps[gb], B_g], w=[B_g])
                      sc.tt("dve", gm[:], ps[:, gb, 0:128], maskm[:], ALU.add, r=[B_ps[gb], B_g], w=[B_g])
                      gn3 = gn[:].rearrange("p (a n) -> p a n", n=8)
                      gm3 = gm[:].rearrange("p (a n) -> p a n", n=8)
                      rk3 = rk[:].rearrange("p (a n) -> p a n", n=8)
                      cm3 = cmpt[:].rearrange("p (a n) -> p a n", n=8)
                      for m in range(7):
                          dst3 = rk3 if m == 0 else cm3
                          sc.tt("dve", dst3, gm3[:, :, m:m + 1].broadcast_to([128, 16, 8]), gn3, ALU.is_gt, r=[B_g],
                                w=[B_g])
                          if m > 0:
                              sc.tt("dve", rk[:], rk[:], cmpt[:], ALU.add, r=[B_g], w=[B_g])
                      sc.ts("dve", bpad[:, :, :, 64:72], rk[:].rearrange("p (q h n) -> p q h n", q=8, h=2), 3.0, NEG,
                            ALU.is_ge, ALU.mult, r=[B_g], w=[B_bpad])
                      for hl in range(2):
                          for gg in range(2):
                              for q4 in range(4):
                                  qt = gg * 4 + q4
                                  sc.mm(ps[0:72, gb, q4 * 128:(q4 + 1) * 128], bpad[:, qt, hl, :], ident[:], True, True,
                                        r=[B_bpad, B_const], w=[B_ps[gb]])
                              sc.cp("dve", QT[hl][64:72, 1024 + gg * 512:1024 + (gg + 1) * 512], ps[64:72, gb, :],
                                    r=[B_ps[gb]], w=[B_QTx[hl]])
                      _ck("mobagate")
                      for hl in range(2):
                          attention(hl, pr, 72, 0.125, oTa, B_oTa)
                      flush_pv()

                  sc.barrier()
                  pm.close()
                  _ck("moba")
                  cqT = sb2("cqT", [128, 3, S], BF16)
                  ckvT = sb2("ckvT", [128, 2, S], BF16)
                  kropeT = sb2("kropeT", [32, S], BF16)
                  sqt = [sb2("sqt0", [128, 512], BF16), sb2("sqt1", [128, 512], BF16)]
                  rbc = sb2("rbc", [128, 512], F32)
                  B_cq = sc.bufs_n("cq", 4)
                  B_ckv = sc.bufs_n("ckv", 4)
                  B_krope = sc.bufs_n("krope", 4)
                  B_sqt = sc.bufs_n("sqt", 2)
                  B_rbc = sc.buf("rbc")
                  wc = sb2("wc", [128, 8, 384], BF16)
                  B_wc = sc.buf("wc")
                  ds_wc = sc.dsem("wc")
                  wuq = sb2("wuq", [128, 3, 256], BF16)
                  wukk = sb2("wukk", [128, 2, 256], BF16)
                  wvb = sb2("wvb", [128, 2, 512], BF16)
                  B_wu = sc.buf("wu")
                  ds_wu = sc.dsem("wu")
                  sc.dma("pool", wc[:, :, 0:384], w_cq_d[:, :, :], ds_wc, w=[B_wc])
                  sc.dma("pool", wvb[:], w_ukvv_d[:, :, :], ds_wv, w=[B_wv])

                  def latent(nch, col0, gcol, dstT, B_dst, g, nfeat):
                      cs = slice(g * 512, (g + 1) * 512)
                      sbank = 7
                      for c in range(nch):
                          b = c
                          for kc in range(8):
                              sc.mm(ps[:, b, :], wc[:, kc, col0 + c * 128:col0 + (c + 1) * 128], hT[:, kc, cs],
                                    kc == 0, kc == 7, r=[B_wc, B_hT[g]], w=[B_ps[b]])
                          sq = sqt[c % 2]
                          sc.act(sq[:], ps[:, b, :], AF.Square, r=[B_ps[b]], w=[B_sqt[c % 2]])
                          sc.mm(ps[:, sbank, :], ones_bf[:], sq[:], c == 0, c == nch - 1, r=[B_const, B_sqt[c % 2]],
                                w=[B_ps[sbank]])
                      sc.act(rbc[:], ps[:, sbank, :], AF.Sqrt, r=[B_ps[sbank], B_const], w=[B_rbc], bias=fcol[:, 4:5],
                             scale=1.0 / nfeat)
                      sc.op("dve", partial(nc.vector.reciprocal, rbc[:], rbc[:]), r=[B_rbc], w=[B_rbc])
                      for c in range(nch):
                          sc.stt("dve", dstT[:, c, cs], ps[:, c, :], gcol[:, c:c + 1], rbc[:], ALU.mult, ALU.mult,
                                 r=[B_ps[c], B_rbc, B_const], w=[B_dst[g]])

                  for g in range(4):
                      latent(3, 0, gq, cqT, B_cq, g, 384.0)
                  sc.dma("pool", wc[:, :, 0:256], w_ckv_d[:, :, :], ds_wc, w=[B_wc])
                  for g in range(4):
                      latent(2, 0, gkv, ckvT, B_ckv, g, 256.0)
                  sc.dma("pool", wc[:, :, 0:64], w_kr_d[:, :, :], ds_wc, w=[B_wc])
                  for g in range(4):
                      b = next_pbank()
                      for kc in range(8):
                          sc.mm(ps[0:64, b, :], wc[:, kc, 0:64], hT[:, kc, g * 512:(g + 1) * 512], kc == 0, kc == 7,
                                r=[B_wc, B_hT[g]], w=[B_ps[b]])
                      rope_rows(kropeT, 32, ps[:, b, :], 32, 32, g, B_krope[g])
                  _ck("mlaprep")
                  for tt in range(16):
                      b = next_pbank()
                      for kc in range(2):
                          sc.mm(ps[:, b, :], ckvT[:, kc, tt * 128:(tt + 1) * 128], wvb[:, kc, :], kc == 0, kc == 1,
                                r=[B_ckv[tt // 4], B_wv], w=[B_ps[b]])
                      src = ps[:, b, :].rearrange("p (a e d) -> p a e d", a=4, e=2)
                      sc.act(Vaug[:, tt, :, 0:64], src[:, :, 0, :], AF.Copy, r=[B_ps[b]], w=[B_V[tt]])
                      sc.cp("dve", Vaug[:, tt, :, 128:192], src[:, :, 1, :], r=[B_ps[b]], w=[B_V[tt]])
                  _ck("mlaV")
                  for pr in range(4):
                      sc.dma("pool", wuq[:], w_uq_d[:, :, pr * 256:(pr + 1) * 256], ds_wu, w=[B_wu])
                      sc.dma("pool", wukk[:], w_ukvk_d[:, :, pr * 256:(pr + 1) * 256], ds_wu, w=[B_wu])
                      _ck("mp1")
                      for hl in range(2):
                          h = 2 * pr + hl
                          for g in range(4):
                              cs = slice(g * 512, (g + 1) * 512)
                              b = next_pbank()
                              for kc in range(3):
                                  sc.mm(ps[:, b, :], wuq[:, kc, hl * 128:(hl + 1) * 128], cqT[:, kc, cs], kc == 0, kc == 2,
                                        r=[B_wu, B_cq[g]], w=[B_ps[b]])
                              _ck("mp2a")
                              sc.act(QT[hl][:, cs], ps[:, b, :], AF.Copy, r=[B_ps[b]], w=[B_QT[hl][g], B_QTx[hl]])
                              _ck("mp2")
                              rope_rows(QT[hl], 32, ps[:, b, :], 32, 32, g, B_QT[hl][g])
                              _ck("mp3")
                              b = next_pbank()
                              for kc in range(2):
                                  sc.mm(ps[:, b, :], wukk[:, kc, hl * 128:(hl + 1) * 128], ckvT[:, kc, cs], kc == 0,
                                        kc == 1, r=[B_wu, B_ckv[g]], w=[B_ps[b]])
                              sc.act(KT[hl][:, cs], ps[:, b, :], AF.Copy, r=[B_ps[b]], w=[B_KT[hl][g], B_KTx[hl]])
                              _ck("mp4")
                              sc.cp("dve", KT[hl][0:32, cs], kropeT[:, cs], r=[B_krope[g]], w=[B_KT[hl][g]])
                              _ck("mp5")
                      _ck("mlaproj")
                      for hl in range(2):
                          attention(hl, pr, 128, 96.0 ** -0.5, oTb, B_oTb)
                      flush_pv()
                  sc.barrier()
                  if dbg and s == 0:
                      for name in dbg:
                          src = {"hT": hT, "oTa": oTa, "oTb": oTb}[name]
                          for kc in range(dbg[name][0] // 128):
                              sc.dma("sp", dbg_d[name][kc * 128:(kc + 1) * 128, :], src[:, kc, :], out_sem[0], w=[])
                  sc.barrier()

              _ck("phase2")
              with ExitStack() as p3:
                  def sb3(name, shape, dt):
                      return sb(name, shape, dt, p3)

                  mergedT = sb3("mergedT", [128, 8, S], BF16)
                  gpm_bc = sb3("gpm_bc", [128, 1024], F32)
                  B_gbc = sc.buf("gbc")
                  sc.dma("sp", gpm_bc[:], gpm_d[0:1, :].partition_broadcast(128), sc.dsem("gbc"), w=[B_gbc])
                  B_mg = sc.bufs_n("mg", 4)
                  wo = sb3("wo", [128, 8, 1024], BF16)
                  B_wo = sc.buf("wo")
                  ds_wo = sc.dsem("wo")
                  wg = [sb3("wg0", [128, 8, 256], BF16), sb3("wg1", [128, 8, 256], BF16)]
                  wab = [sb3("wab0", [128, 4, 256], BF16), sb3("wab1", [128, 4, 256], BF16)]
                  B_wg = sc.bufs_n("wg", 2)
                  ds_wg = [sc.dsem("wg0"), sc.dsem("wg1")]
                  sig = [[sb3("sig%d%d" % (a, b), [128, 512], F32) for b in range(2)] for a in range(2)]
                  mt = [[sb3("mt%d%d" % (a, b), [128, 512], F32) for b in range(2)] for a in range(2)]
                  B_sig = [sc.bufs_n("sig%d_" % a, 2) for a in range(2)]
                  B_mt = [sc.bufs_n("mt%d_" % a, 2) for a in range(2)]
                  ysb = sb3("ysb", [128, 1024], F32)
                  tmpy = sb3("tmpy", [128, 1024], F32)
                  junk3 = sb3("junk3", [128, 1024], F32)
                  xt3 = [sb3("xt3_0", [128, 1024], F32), sb3("xt3_1", [128, 1024], F32)]
                  x1t = [sb3("x1t0", [128, 1024], F32), sb3("x1t1", [128, 1024], F32)]
                  st3 = sb3("st3", [128, 2], F32)
                  B_ysb = sc.buf("ysb")
                  B_tmpy = sc.buf("tmpy")
                  B_junk3 = sc.buf("junk3")
                  B_xt3 = sc.bufs_n("xt3", 2)
                  B_x1t = sc.bufs_n("x1t", 2)
                  B_st3 = sc.buf("st3")

                  def load_w3(fo, sl):
                      sc.dma("pool", wg[sl][:, :, 0:128], w_gate_d[:, :, fo * 128:(fo + 1) * 128], ds_wg[sl],
                             w=[B_wg[sl]])
                      sc.dma("pool", wg[sl][:, :, 128:256], w_gate_d[:, :, 1024 + fo * 128:1024 + (fo + 1) * 128],
                             ds_wg[sl], w=[B_wg[sl]])
                      sc.dma("pool", wab[sl][:, :, 0:128], w_a_d[:, :, fo * 128:(fo + 1) * 128], ds_wg[sl],
                             w=[B_wg[sl]])
                      sc.dma("pool", wab[sl][:, :, 128:256], w_b_d[:, :, fo * 128:(fo + 1) * 128], ds_wg[sl],
                             w=[B_wg[sl]])

                  load_w3(0, 0)
                  sc.dma("pool", wo[:], w_out_d[:, :, :], ds_wo, w=[B_wo])
                  it = 0
                  for fo in range(8):
                      sl = fo % 2
                      if fo + 1 < 8:
                          load_w3(fo + 1, 1 - sl)
                      for g in range(4):
                          cs = slice(g * 512, (g + 1) * 512)
                          a = it % 2
                          it += 1
                          bb = 4 * a
                          for half in range(2):
                              for kc in range(8):
                                  sc.mm(ps[:, bb + half, :], wg[sl][:, kc, half * 128:(half + 1) * 128], hT[:, kc, cs],
                                        kc == 0, kc == 7, r=[B_wg[sl], B_hT[g]], w=[B_ps[bb + half]])
                              sc.act(sig[a][half][:], ps[:, bb + half, :], AF.Sigmoid, r=[B_ps[bb + half], B_const],
                                     w=[B_sig[a][half]], bias=bgate[:, half * 8 + fo:half * 8 + fo + 1], scale=1.0)
                          for half in range(2):
                              oT = oTa if half == 0 else oTb
                              B_oT = B_oTa if half == 0 else B_oTb
                              for pr in range(4):
                                  sc.mm(ps[:, bb + 2 + half, :], wab[sl][:, pr, half * 128:(half + 1) * 128],
                                        oT[:, pr, cs], pr == 0, pr == 3, r=[B_wg[sl], B_oT[pr][g]],
                                        w=[B_ps[bb + 2 + half]])
                              sc.tt("dve", mt[a][half][:], ps[:, bb + 2 + half, :], sig[a][half][:], ALU.mult,
                                    r=[B_ps[bb + 2 + half], B_sig[a][half]], w=[B_mt[a][half]])
                          sc.tt("pool", mergedT[:, fo, cs], mt[a][0][:], mt[a][1][:], ALU.add,
                                r=[B_mt[a][0], B_mt[a][1]], w=[B_mg[g]])
                  for tt in range(16):
                      sl = tt % 2
                      yb = 2 * sl
                      sc.dma("sp", xt3[sl][:], x_d[r0 + tt * 128:r0 + (tt + 1) * 128, :], xt_sem[sl], w=[B_xt3[sl]])
                      for half in range(2):
                          for fo in range(8):
                              sc.mm(ps[:, yb + half, :], mergedT[:, fo, tt * 128:(tt + 1) * 128],
                                    wo[:, fo, half * 512:(half + 1) * 512], fo == 0, fo == 7, r=[B_mg[tt // 4], B_wo],
                                    w=[B_ps[yb + half]])
                      sc.act(ysb[:, 0:512], ps[:, yb, :], AF.Copy, r=[B_ps[yb]], w=[B_ysb])
                      sc.act(ysb[:, 512:1024], ps[:, yb + 1, :], AF.Copy, r=[B_ps[yb + 1]], w=[B_ysb])
                      sc.stt("dve", junk3[:], ysb[:], 1.0, ysb[:], ALU.mult, ALU.mult, r=[B_ysb], w=[B_junk3, B_st3],
                             accum_out=st3[:, 0:1])
                      sc.act(st3[:, 1:2], st3[:, 0:1], AF.Sqrt, r=[B_st3, B_const], w=[B_st3], bias=fcol[:, 4:5], scale=1.0 / D)
                      sc.op("dve", partial(nc.vector.reciprocal, st3[:, 1:2], st3[:, 1:2]), r=[B_st3], w=[B_st3])
                      sc.stt("dve", tmpy[:], ysb[:], st3[:, 1:2], gpm_bc[:], ALU.mult, ALU.mult,
                             r=[B_ysb, B_st3, B_gbc], w=[B_tmpy])
                      sc.tt("pool", x1t[sl][:], tmpy[:], xt3[sl][:], ALU.add, r=[B_tmpy, B_xt3[sl]], w=[B_x1t[sl]])
                      sc.dma("sp", out_d[r0 + tt * 128:r0 + (tt + 1) * 128, :], x1t[sl][:], out_sem[sl],
                             r=[B_x1t[sl]], w=[B_x1d[s][tt]])
                  sc.barrier()

              _ck("phase3")
              with ExitStack() as p4:
                  def sb4(name, shape, dt):
                      return sb(name, shape, dt, p4)

                  wdn = sb4("wdn", [128, 32, 1024], BF16)
                  gpl_bc = sb4("gpl_bc", [128, 1024], F32)
                  B_gbc4 = sc.buf("gbc4")
                  sc.dma("sp", gpl_bc[:], gpl_d[0:1, :].partition_broadcast(128), sc.dsem("gbc"), w=[B_gbc4])
                  B_wdn = sc.bufs_n("wdn", 4)
                  ds_wdn = sc.dsem("wdn")
                  wup = [oTa[:, i, :].rearrange("p (k n) -> p k n", n=256) for i in range(4)]
                  B_wup = sc.bufs_n("wup", 4)
                  ds_wup = [sc.dsem("wup%d" % i) for i in range(4)]
                  def aTs(uc, lo=0, hi=512):
                      return hT[:, uc // 4, (uc % 4) * 512 + lo:(uc % 4) * 512 + hi]

                  B_aT = sc.bufs_n("aT", 32)
                  h2T = oTb[:, 0:2, :].rearrange("p a (k n) -> p (a k) n", n=512)
                  B_h2T = sc.buf("h2T")
                  x1g = [sb4("x1g%d" % i, [128, 1024], F32) for i in range(4)]
                  B_x1g = sc.bufs_n("x1g", 4)
                  xn4 = [oTb[:, 2, 0:1024], oTb[:, 2, 1024:2048]]
                  B_xn4 = sc.bufs_n("xn4", 2)
                  junk4 = sb4("junk4", [128, 1024], F32)
                  B_junk4 = sc.buf("junk4")
                  st4 = sb4("st4", [128, 4], F32)
                  B_st4 = sc.bufs_n("st4", 2)
                  rl = [sb4("rl0", [128, 512], F32), sb4("rl1", [128, 512], F32)]
                  B_rl = sc.bufs_n("rl", 2)
                  ysb4 = sb4("ysb4", [128, 1024], F32)
                  tmp4 = sb4("tmp4", [128, 1024], F32)
                  ot = [sb4("ot0", [128, 1024], F32), sb4("ot1", [128, 1024], F32)]
                  B_ysb4 = sc.buf("ysb4")
                  B_tmp4 = sc.buf("tmp4")
                  B_ot = sc.bufs_n("ot", 2)
                  ds_x1g = [sc.dsem("x1g%d" % i) for i in range(4)]

                  def load_up(c):
                      sc.dma("pool", wup[c % 4], w_up_d[:, :, c * 256:(c + 1) * 256], ds_wup[c % 4], w=[B_wup[c % 4]])

                  for c in range(3):
                      load_up(c)
                  for g in range(4):
                      for j in range(4):
                          tt = g * 4 + j
                          sc.dma("sp", x1g[j][:], out_d[r0 + tt * 128:r0 + (tt + 1) * 128, :], ds_x1g[j],
                                 r=[B_x1d[s][tt]], w=[B_x1g[j]])
                          sl = j % 2
                          norm_transpose(x1g[j][:], B_x1g[j], j * 128, xn4[sl], B_xn4[sl], st4[:, sl:sl + 1],
                                         st4[:, 2 + sl:3 + sl], B_st4[sl], junk4, B_junk4, sl, gpre2, h2T, B_h2T)
                      for c in range(16):
                          sl = c % 4
                          if c + 3 < 16:
                              load_up(c + 3)
                          if g == 0 and c % 3 == 0 and c // 3 < 4:
                              kcg = c // 3
                              sc.dma("pool", wdn[:, kcg * 8:(kcg + 1) * 8, :], w_down_d[:, kcg * 8:(kcg + 1) * 8, :],
                                     ds_wdn, w=[B_wdn[kcg]])
                          for ucl in range(2):
                              uc = c * 2 + ucl
                              b = 2 + (uc % 2)
                              for kc in range(8):
                                  sc.mm(ps[:, b, :], wup[sl][:, kc, ucl * 128:(ucl + 1) * 128], h2T[:, kc, :], kc == 0,
                                        kc == 7, r=[B_wup[sl], B_h2T], w=[B_ps[b]])
                              sc.act(rl[uc % 2][:], ps[:, b, :], AF.Relu, r=[B_ps[b]], w=[B_rl[uc % 2]])
                              sc.tt("pool", aTs(uc), rl[uc % 2][:], rl[uc % 2][:], ALU.mult, r=[B_rl[uc % 2]],
                                    w=[B_aT[uc]])
                      if g + 1 < 4:
                          for c in range(3):
                              load_up(c)
                      for j in range(4):
                          tt = g * 4 + j
                          yb = 4 + 2 * (j % 2)
                          for half in range(2):
                              for uc in range(32):
                                  sc.mm(ps[:, yb + half, :], aTs(uc, j * 128, (j + 1) * 128),
                                        wdn[:, uc, half * 512:(half + 1) * 512], uc == 0, uc == 31,
                                        r=[B_aT[uc], B_wdn[uc // 8]], w=[B_ps[yb + half]])
                          sc.act(ysb4[:, 0:512], ps[:, yb, :], AF.Copy, r=[B_ps[yb]], w=[B_ysb4])
                          sc.act(ysb4[:, 512:1024], ps[:, yb + 1, :], AF.Copy, r=[B_ps[yb + 1]], w=[B_ysb4])
                          sc.stt("dve", junk4[:], ysb4[:], 1.0, ysb4[:], ALU.mult, ALU.mult, r=[B_ysb4],
                                 w=[B_junk4, B_st4[0]], accum_out=st4[:, 0:1])
                          sc.act(st4[:, 2:3], st4[:, 0:1], AF.Sqrt, r=[B_st4[0], B_const], w=[B_st4[0]], bias=fcol[:, 4:5], scale=1.0 / D)
                          sc.op("dve", partial(nc.vector.reciprocal, st4[:, 2:3], st4[:, 2:3]), r=[B_st4[0]], w=[B_st4[0]])
                          sc.stt("dve", tmp4[:], ysb4[:], st4[:, 2:3], gpl_bc[:], ALU.mult, ALU.mult,
                                 r=[B_ysb4, B_st4[0], B_gbc4], w=[B_tmp4])
                          sc.tt("pool", ot[j % 2][:], tmp4[:], x1g[j][:], ALU.add, r=[B_tmp4, B_x1g[j]], w=[B_ot[j % 2]])
                          sc.dma("sp", out_d[r0 + tt * 128:r0 + (tt + 1) * 128, :], ot[j % 2][:], out_sem[j % 2],
                                 r=[B_ot[j % 2]], w=[B_x1d[s][tt]])
                  sc.barrier()

        except _Stop:
            raise
        n_ins, n_wait = sc.emit()
        build_nc.stats = (n_ins, n_wait)
    return nc


_NC_CACHE = {}


def kernel(x, positions, g_pre_mix, w_in, b_gate, g_q_norm, w_uq, g_kv_norm, w_ukv, w_branch_a, w_branch_b, w_out,
           g_post_mix, g_pre_mlp, w_up, w_down, g_post_mlp):
    x = np.asarray(x, dtype=np.float32)
    positions = np.asarray(positions, dtype=np.int32)
    args = [np.asarray(a, dtype=np.float32) for a in
            (w_in, w_uq, w_ukv, w_branch_a, w_branch_b, w_out, w_up, w_down, g_pre_mix, b_gate, g_q_norm,
             g_kv_norm, g_post_mix, g_pre_mlp, g_post_mlp)]
    shared = _host_weights(*args)
    shared.update(_host_consts())
    if "nc" not in _NC_CACHE:
        _NC_CACHE["nc"] = build_nc(NSEQ)
    nc = _NC_CACHE["nc"]
    in_maps = []
    for c in range(NCORES):
        m = dict(shared)
        m["x"] = np.ascontiguousarray(x[c * NSEQ:(c + 1) * NSEQ].reshape(NSEQ * S, D))
        m["pos"] = np.ascontiguousarray(positions[c * NSEQ:(c + 1) * NSEQ])
        in_maps.append(m)
    res = run_bass_kernel_spmd(nc, in_maps, core_ids=list(range(NCORES)))
    out = np.concatenate([np.asarray(r["out"]).reshape(NSEQ, S, D) for r in res.results], axis=0)
    return out.astype(np.float32, copy=False)
```
